# Optimizing a Trainium2 kernel written in Bass

```python
import math, functools
import jax, jax.numpy as jnp
from jax import lax
import numpy as np

D_MODEL = 2048
BATCH = 8
SEQ = 2048
DEPTH = 1

MIX_WIDTH = D_MODEL
POOL_WIDTH = MIX_WIDTH // 2
POOL_GROUPS = 4
POOL_GROUP_DIM = POOL_WIDTH // POOL_GROUPS
POOL_WINDOWS = (2, 4, 8, 16)
DN_WIDTH = MIX_WIDTH - POOL_WIDTH
DN_HEAD_DIM = 128
DN_HEADS = DN_WIDTH // DN_HEAD_DIM
CONV_WIDTH = 4
CHUNK = 64
IN_COLS = POOL_WIDTH + 4 * DN_WIDTH + 2 * DN_HEADS
N_GROUPS = 4
EXPERTS_PER_GROUP = 8
N_EXPERTS = N_GROUPS * EXPERTS_PER_GROUP
TOP_K = 2
D_EXPERT = 768
MOE_BLOCK = 128
EPS = 1e-6

kernel_name = "hybrid_pool_deltanet_hmoe_adaln"


def rmsnorm(x, g):
    xf = x.astype(jnp.float32)
    y = xf * lax.rsqrt(jnp.mean(xf * xf, axis=-1, keepdims=True) + EPS)
    return (y * g.astype(jnp.float32)).astype(x.dtype)


def l2norm(x):
    return x * lax.rsqrt(jnp.sum(x * x, axis=-1, keepdims=True) + EPS)


def pool_mixer(u, pool_w, pool_scale):
    B, S, _ = u.shape
    uf = u.reshape(B, S, POOL_GROUPS, POOL_GROUP_DIM).astype(jnp.float32)
    cs = jnp.concatenate([jnp.zeros((B, 1, POOL_GROUPS, POOL_GROUP_DIM), jnp.float32),
                          jnp.cumsum(uf, axis=1)], axis=1)
    win = jnp.array(POOL_WINDOWS, jnp.int32)
    t1 = jnp.arange(1, S + 1, dtype=jnp.int32)[:, None]
    lo = jnp.maximum(t1 - win[None, :], 0)
    cnt = (t1 - lo).astype(jnp.float32)
    lower = cs[:, lo, jnp.arange(POOL_GROUPS)[None, :]]
    mean = (cs[:, 1:] - lower) / cnt[None, :, :, None]
    diff = (mean - uf).astype(u.dtype)
    y = jnp.einsum('bsgc,gcd->bsgd', diff, pool_w)
    return y.reshape(B, S, POOL_WIDTH) * pool_scale


def causal_depthwise_conv(x, w):
    C = x.shape[-1]
    return lax.conv_general_dilated(x, w[:, None, :], window_strides=(1,),
                                    padding=[(CONV_WIDTH - 1, 0)],
                                    dimension_numbers=('NWC', 'WIO', 'NWC'),
                                    feature_group_count=C)


def chunked_gated_delta_rule(q, k, v, g, beta):
    B, H, S, dk = q.shape
    dv = v.shape[-1]
    n = S // CHUNK

    def chunks(t):
        return t.reshape(B, H, n, CHUNK, *t.shape[3:])

    q = chunks(q * dk ** -0.5)
    k = chunks(k)
    v = chunks(v)
    beta = chunks(beta)
    g = jnp.cumsum(chunks(g), axis=-1)
    incl = jnp.tril(jnp.ones((CHUNK, CHUNK), bool))
    strict = jnp.tril(jnp.ones((CHUNK, CHUNK), bool), -1)
    diff = g[..., :, None] - g[..., None, :]
    decay = jnp.where(incl, jnp.exp(jnp.where(incl, diff, 0.0)), 0.0)
    k_beta = k * beta[..., None]
    a_mat = jnp.eye(CHUNK, dtype=q.dtype) + jnp.where(
        strict, jnp.einsum('bhnid,bhnjd->bhnij', k_beta, k) * decay, 0.0)
    solve = functools.partial(lax.linalg.triangular_solve, left_side=True, lower=True,
                              unit_diagonal=True)
    u = solve(a_mat, v * beta[..., None])
    w = solve(a_mat, k_beta * jnp.exp(g)[..., None])
    qk = jnp.where(incl, jnp.einsum('bhnid,bhnjd->bhnij', q, k) * decay, 0.0)

    def step(state, xs):
        q_c, k_c, u_c, w_c, qk_c, g_c = xs
        v_new = u_c - jnp.einsum('bhcd,bhde->bhce', w_c, state)
        o_c = (jnp.einsum('bhcd,bhde->bhce', q_c * jnp.exp(g_c)[..., None], state)
               + jnp.einsum('bhij,bhje->bhie', qk_c, v_new))
        g_last = g_c[..., -1:]
        state = (state * jnp.exp(g_last)[..., None]
                 + jnp.einsum('bhcd,bhce->bhde', k_c * jnp.exp(g_last - g_c)[..., None], v_new))
        return state, o_c

    xs = tuple(jnp.moveaxis(t, 2, 0) for t in (q, k, u, w, qk, g))
    state0 = jnp.zeros((B, H, dk, dv), jnp.float32)
    _, o = lax.scan(step, state0, xs)
    return jnp.moveaxis(o, 0, 2).reshape(B, H, S, dv)


def gated_deltanet(qkv, z, beta_logit, a_logit, conv_w, a_log, dt_bias, o_norm_g):
    B, S, _ = qkv.shape
    dtype = qkv.dtype
    qkv = jax.nn.silu(causal_depthwise_conv(qkv, conv_w))
    q, k, v = jnp.split(qkv, 3, axis=-1)

    def heads(t):
        return t.reshape(B, S, DN_HEADS, DN_HEAD_DIM).transpose(0, 2, 1, 3).astype(jnp.float32)

    q = l2norm(heads(q))
    k = l2norm(heads(k))
    v = heads(v)
    beta = jax.nn.sigmoid(beta_logit.astype(jnp.float32)).transpose(0, 2, 1)
    g = (-jnp.exp(a_log.astype(jnp.float32))
         * jax.nn.softplus(a_logit.astype(jnp.float32) + dt_bias.astype(jnp.float32))
         ).transpose(0, 2, 1)
    o = chunked_gated_delta_rule(q, k, v, g, beta).transpose(0, 2, 1, 3)
    o = o * lax.rsqrt(jnp.mean(o * o, axis=-1, keepdims=True) + EPS) * o_norm_g.astype(jnp.float32)
    o = o * jax.nn.silu(z.reshape(B, S, DN_HEADS, DN_HEAD_DIM).astype(jnp.float32))
    return o.reshape(B, S, DN_WIDTH).astype(dtype)


def hybrid_mixer(h, w_in, pool_w, pool_scale, conv_w, a_log, dt_bias, o_norm_g, w_out):
    proj = h @ w_in
    u, qkv, z, beta_logit, a_logit = jnp.split(
        proj, [POOL_WIDTH, POOL_WIDTH + 3 * DN_WIDTH, POOL_WIDTH + 4 * DN_WIDTH,
               POOL_WIDTH + 4 * DN_WIDTH + DN_HEADS], axis=-1)
    y_pool = pool_mixer(u, pool_w, pool_scale)
    y_dn = gated_deltanet(qkv, z, beta_logit, a_logit, conv_w, a_log, dt_bias, o_norm_g)
    return jnp.concatenate([y_pool, y_dn], axis=-1) @ w_out


def routed_experts(ht, expert_idx, expert_w, w_gate, w_up, w_down):
    T, D = ht.shape
    A = T * TOP_K
    n_blocks = -(-(A + N_EXPERTS * (MOE_BLOCK - 1)) // MOE_BLOCK)
    n_pad = n_blocks * MOE_BLOCK
    flat_e = expert_idx.reshape(A)
    flat_tok = jnp.repeat(jnp.arange(T, dtype=jnp.int32), TOP_K)
    flat_w = expert_w.reshape(A)
    order = jnp.argsort(flat_e)
    e_sorted = flat_e[order]
    counts = jnp.bincount(flat_e, length=N_EXPERTS)
    padded = (counts + MOE_BLOCK - 1) // MOE_BLOCK * MOE_BLOCK
    pad_end = jnp.cumsum(padded)
    pad_start = pad_end - padded
    start = jnp.cumsum(counts) - counts
    dest = pad_start[e_sorted] + jnp.arange(A, dtype=jnp.int32) - start[e_sorted]
    buf_tok = jnp.zeros((n_pad,), jnp.int32).at[dest].set(flat_tok[order])
    buf_w = jnp.zeros((n_pad,), ht.dtype).at[dest].set(flat_w[order])
    block_start = jnp.arange(n_blocks, dtype=pad_end.dtype) * MOE_BLOCK
    block_e = jnp.minimum(jnp.searchsorted(pad_end, block_start, side='right'), N_EXPERTS - 1)

    def block_fn(args):
        tok_b, w_b, e_b = args
        xb = ht[tok_b]
        hid = jax.nn.silu(xb @ w_gate[e_b]) * (xb @ w_up[e_b])
        return (hid @ w_down[e_b]) * w_b[:, None]

    ys = lax.map(block_fn, (buf_tok.reshape(n_blocks, MOE_BLOCK),
                            buf_w.reshape(n_blocks, MOE_BLOCK), block_e))
    return jnp.zeros((T, D), ht.dtype).at[buf_tok].add(ys.reshape(n_pad, D))


def hierarchical_moe(h, w_rg, b_rg, w_re, b_re, w_gate, w_up, w_down):
    B, S, D = h.shape
    T = B * S
    ht = h.reshape(T, D)
    hf = ht.astype(jnp.float32)
    p_group = jax.nn.softmax(hf @ w_rg.astype(jnp.float32) + b_rg.astype(jnp.float32), axis=-1)
    p_top, g_idx = lax.top_k(p_group, 1)
    logit_e = (hf @ w_re.astype(jnp.float32) + b_re.astype(jnp.float32)).reshape(
        T, N_GROUPS, EXPERTS_PER_GROUP)
    logit_in = logit_e[jnp.arange(T), g_idx[:, 0]]
    top_logit, e_local = lax.top_k(logit_in, TOP_K)
    weights = p_top * jax.nn.softmax(top_logit, axis=-1)
    expert_idx = g_idx * EXPERTS_PER_GROUP + e_local
    y = routed_experts(ht, expert_idx, weights.astype(h.dtype), w_gate, w_up, w_down)
    return y.reshape(B, S, D)


def setup_inputs(seed: int = 0) -> dict:
    key = jax.random.key(seed)
    ks = jax.random.split(key, 24)
    f32 = jnp.float32
    L = DEPTH

    def nrm(k, shape, scale):
        return jax.random.normal(k, shape, f32) * scale

    dt = jnp.exp(jax.random.uniform(ks[10], (L, DN_HEADS), f32,
                                    minval=math.log(1e-3), maxval=math.log(1e-1)))
    return {
        "x": nrm(ks[0], (BATCH, SEQ, D_MODEL), 1.0),
        "c": nrm(ks[1], (BATCH, D_MODEL), 1.0),
        "w_ada": nrm(ks[2], (L, D_MODEL, 6 * D_MODEL), D_MODEL ** -0.5),
        "b_ada": nrm(ks[3], (L, 6 * D_MODEL), 0.02),
        "norm1_g": 1.0 + nrm(ks[4], (L, D_MODEL), 0.02),
        "w_in": nrm(ks[5], (L, D_MODEL, IN_COLS), D_MODEL ** -0.5),
        "pool_w": nrm(ks[6], (L, POOL_GROUPS, POOL_GROUP_DIM, POOL_GROUP_DIM), POOL_GROUP_DIM ** -0.5),
        "pool_scale": 1.0 + nrm(ks[7], (L, POOL_WIDTH), 0.02),
        "conv_w": nrm(ks[8], (L, CONV_WIDTH, 3 * DN_WIDTH), CONV_WIDTH ** -0.5),
        "a_log": jnp.log(jax.random.uniform(ks[9], (L, DN_HEADS), f32, minval=1.0, maxval=16.0)),
        "dt_bias": dt + jnp.log(-jnp.expm1(-dt)),
        "o_norm_g": 1.0 + nrm(ks[11], (L, DN_HEAD_DIM), 0.02),
        "w_out": nrm(ks[12], (L, MIX_WIDTH, D_MODEL), MIX_WIDTH ** -0.5),
        "norm2_g": 1.0 + nrm(ks[13], (L, D_MODEL), 0.02),
        "w_router_group": nrm(ks[14], (L, D_MODEL, N_GROUPS), D_MODEL ** -0.5),
        "b_router_group": nrm(ks[15], (L, N_GROUPS), 0.01),
        "w_router_expert": nrm(ks[16], (L, D_MODEL, N_EXPERTS), D_MODEL ** -0.5),
        "b_router_expert": nrm(ks[17], (L, N_EXPERTS), 0.01),
        "w_gate": nrm(ks[18], (L, N_EXPERTS, D_MODEL, D_EXPERT), D_MODEL ** -0.5),
        "w_up": nrm(ks[19], (L, N_EXPERTS, D_MODEL, D_EXPERT), D_MODEL ** -0.5),
        "w_down": nrm(ks[20], (L, N_EXPERTS, D_EXPERT, D_MODEL), D_EXPERT ** -0.5),
        "norm_f_g": 1.0 + nrm(ks[21], (D_MODEL,), 0.02),
    }


def reference(x, c, w_ada, b_ada, norm1_g, w_in, pool_w, pool_scale, conv_w, a_log, dt_bias,
              o_norm_g, w_out, norm2_g, w_router_group, b_router_group, w_router_expert,
              b_router_expert, w_gate, w_up, w_down, norm_f_g):
    c_act = jax.nn.silu(c)
    for l in range(DEPTH):
        mod = c_act @ w_ada[l] + b_ada[l]
        shift1, scale1, gate1, shift2, scale2, gate2 = [m[:, None, :] for m in jnp.split(mod, 6, axis=-1)]
        h = rmsnorm(x, norm1_g[l]) * (1.0 + scale1) + shift1
        x = x + gate1 * hybrid_mixer(h, w_in[l], pool_w[l], pool_scale[l], conv_w[l], a_log[l],
                                     dt_bias[l], o_norm_g[l], w_out[l])
        h = rmsnorm(x, norm2_g[l]) * (1.0 + scale2) + shift2
        x = x + gate2 * hierarchical_moe(h, w_router_group[l], b_router_group[l], w_router_expert[l],
                                         b_router_expert[l], w_gate[l], w_up[l], w_down[l])
    return rmsnorm(x, norm_f_g)
```

```python
import contextlib
from collections import defaultdict

import numpy as np
import concourse.bass as bass
import concourse.mybir as mybir
from concourse.bass_utils import run_bass_kernel_spmd

F32 = mybir.dt.float32
BF16 = mybir.dt.bfloat16
I32 = mybir.dt.int32
ALU = mybir.AluOpType
AF = mybir.ActivationFunctionType

T = 2048
D = 2048
NT = 16
NE = 32
CAP = 256
DE = 768
EPS = 1e-6
SAME_SYNC = True


class Res:
    __slots__ = ("lw", "rd")

    def __init__(self):
        self.lw = None
        self.rd = {}


class Prog:
    ENGS = ("pe", "act", "dve", "pool", "sp")

    def __init__(self):
        self.q = {e: [] for e in self.ENGS}
        self.cnt = defaultdict(int)
        self.known = {e: defaultdict(int) for e in self.ENGS}
        self.dma_sems = {"sp": ["dsp%d" % i for i in range(8)],
                         "pool": ["dpl%d" % i for i in range(8)],
                         "act": ["dac%d" % i for i in range(4)]}
        self.rr = defaultdict(int)
        self.mult = {}
        for e in ("pe", "act", "dve", "pool"):
            self.mult[e] = 1
        for l in self.dma_sems.values():
            for s in l:
                self.mult[s] = 16

    def _need(self, eng, waits, dep):
        ctr, n = dep
        if ctr == eng and (eng == "pe" or not SAME_SYNC):
            return
        if self.known[eng][ctr] >= n:
            return
        if waits.get(ctr, 0) < n:
            waits[ctr] = n

    def _deps(self, eng, reads, writes):
        waits = {}
        for r in reads:
            if r.lw is not None:
                self._need(eng, waits, r.lw)
        for r in writes:
            if r.lw is not None:
                self._need(eng, waits, r.lw)
            for c, n in r.rd.items():
                self._need(eng, waits, (c, n))
        return waits

    def op(self, eng, fn, reads=(), writes=()):
        waits = self._deps(eng, reads, writes)
        for c, n in waits.items():
            self.known[eng][c] = n
        self.cnt[eng] += 1
        n = self.cnt[eng]
        self.q[eng].append((waits, fn, eng, 1))
        for r in reads:
            if r.rd.get(eng, 0) < n:
                r.rd[eng] = n
        for r in writes:
            r.lw = (eng, n)
            r.rd = {}

    def dma(self, eng, fn, reads=(), writes=()):
        names = self.dma_sems[eng]
        ctr = names[self.rr[eng] % len(names)]
        self.rr[eng] += 1
        waits = self._deps(eng, reads, writes)
        if self.cnt[ctr] > 0:
            self._need(eng, waits, (ctr, self.cnt[ctr]))
        for c, n in waits.items():
            self.known[eng][c] = n
        self.cnt[ctr] += 1
        n = self.cnt[ctr]
        self.q[eng].append((waits, fn, ctr, 16))
        for r in reads:
            r.rd[ctr] = n
        for r in writes:
            r.lw = (ctr, n)
            r.rd = {}

    def barrier(self):
        snap = dict(self.cnt)
        for eng in self.ENGS:
            waits = {}
            for c, n in snap.items():
                if n == 0 or (c == eng and eng == "pe"):
                    continue
                if self.known[eng][c] < n:
                    waits[c] = n
                    self.known[eng][c] = n
            if waits:
                self.q[eng].append((waits, None, None, 0))

    def begin_cond(self, cnt_ap, thr):
        if not hasattr(self, "_cstack"):
            self._cstack = []
        known_snap = {e: dict(k) for e, k in self.known.items()}
        marks = {}
        for eng in self.ENGS:
            m = {"kind": "begin", "ap": cnt_ap, "thr": thr, "pre": dict(self.cnt), "delta": {}}
            marks[eng] = m
            self.q[eng].append((None, m, None, 0))
        self._cstack.append((marks, known_snap))

    def end_cond(self):
        marks, known_snap = self._cstack.pop()
        for eng in self.ENGS:
            m = marks[eng]
            idx = max(i for i, it in enumerate(self.q[eng]) if it[1] is m)
            delta = defaultdict(int)
            mw = {}
            for waits, fn, ctr, inc in self.q[eng][idx + 1:]:
                if ctr is not None and callable(fn):
                    delta[ctr] += 1
                if waits:
                    for c, n in waits.items():
                        n = min(n, m["pre"].get(c, 0))
                        if n > mw.get(c, 0):
                            mw[c] = n
            m["delta"] = dict(delta)
            m["skipwaits"] = mw
            self.q[eng].append((None, {"kind": "end", "begin": m}, None, 0))
        for e in self.ENGS:
            self.known[e] = defaultdict(int, known_snap[e])

    def emit(self, nc, block, sems):
        attr = {"pe": "tensor", "act": "scalar", "dve": "vector", "pool": "gpsimd", "sp": "sync"}
        final = dict(self.cnt)
        for eng in self.ENGS:
            items = self.q[eng]

            def body(e, items=items, eng=eng):
                reg = None
                cms = []
                for waits, fn, ctr, inc in items:
                    if isinstance(fn, dict):
                        if fn["kind"] == "begin":
                            if not fn["delta"]:
                                continue
                            if reg is None:
                                reg = e.alloc_register("cnt_" + eng)
                            e.reg_load(reg, fn["ap"])
                            cm = e.If_lt(reg, fn["thr"])
                            cm.__enter__()
                            for c, n in fn.get("skipwaits", {}).items():
                                if n > 0 and c != eng:
                                    e.wait_ge(sems[c], n * self.mult[c])
                            for c, dn in fn["delta"].items():
                                if fn["pre"].get(c, 0) > 0:
                                    e.wait_ge(sems[c], fn["pre"][c] * self.mult[c])
                                e.sem_inc(sems[c], dn * self.mult[c])
                            cm.__exit__(None, None, None)
                            cm = e.Else()
                            cm.__enter__()
                            cms.append(cm)
                        else:
                            if fn["begin"]["delta"]:
                                cms.pop().__exit__(None, None, None)
                        continue
                    for c, n in waits.items():
                        e.wait_ge(sems[c], n * self.mult[c])
                    if fn is not None:
                        fn(e).then_inc(sems[ctr], inc)
                if eng == "sp":
                    for c, n in final.items():
                        if n > 0:
                            e.wait_ge(sems[c], n * self.mult[c])

            getattr(block, attr[eng])(body)


def make_consts():
    i = np.arange(128)
    cs = {}
    cs["ident"] = np.eye(128, dtype=np.float32)
    cs["uinc"] = (i[:, None] <= i[None, :]).astype(np.float32)
    cs["ustr"] = (i[:, None] < i[None, :]).astype(np.float32)
    cs["mneg"] = np.where(i[None, :] >= i[:, None], 0.0, -30000.0).astype(np.float32)
    cs["ones"] = np.ones((128, 128), np.float32)
    for l in range(1, 8):
        same = (i[:, None] >> l) == (i[None, :] >> l)
        diff = (i[:, None] >> (l - 1)) != (i[None, :] >> (l - 1))
        cs["lm%d" % l] = (same & diff & (i[:, None] < i[None, :])).astype(np.float32)
    cs["iota"] = np.broadcast_to(np.arange(CAP, dtype=np.float32)[None, :], (128, CAP)).copy()
    invc = np.zeros((128, 4, 16), np.float32)
    for g, w in enumerate((2, 4, 8, 16)):
        invc[:, g, :] = 1.0 / np.minimum(np.arange(16) + 1, w)
    cs["invc"] = invc.reshape(128, 64)
    cs["eoff"] = np.broadcast_to((np.arange(NE, dtype=np.float32) * 2048.0)[None, :], (128, NE)).copy()
    cs["b128"] = np.broadcast_to((np.arange(64, dtype=np.float32) * 128.0)[None, :], (128, 64)).copy()
    cs["misc"] = np.broadcast_to(np.array([EPS, 1.0, 0.0, -1.0], np.float32)[None, :], (128, 4)).copy()
    offs = {}
    cols = []
    o = 0
    for k, v in cs.items():
        offs[k] = (o, v.shape[1])
        cols.append(v)
        o += v.shape[1]
    return np.concatenate(cols, axis=1), offs


CONSTS, COFF = make_consts()
NCONST = CONSTS.shape[1]


def build(stage=99):
    nc = bass.Bass("TRN2", target_bir_lowering=False)
    P = Prog()

    def din(name, shape, dt=F32):
        return nc.dram_tensor(name, list(shape), dt, kind="ExternalInput").ap()

    x_d = din("x", [T, D])
    c_d = din("c", [128, 16])
    w_ada = din("w_ada", [D, 6 * D])
    b_ada = din("b_ada", [6 * D])
    norm1_g = din("norm1_g", [D])
    w_in = din("w_in", [D, 5136])
    pool_w = din("pool_w", [4, 256, 256])
    pool_scale = din("pool_scale", [1024])
    conv_w = din("conv_w", [4, 3072])
    a_log = din("a_log", [8])
    dt_bias = din("dt_bias", [8])
    o_norm_g = din("o_norm_g", [128])
    w_out = din("w_out", [D, D])
    norm2_g = din("norm2_g", [D])
    w_rg = din("w_rg", [D, 4])
    b_rg = din("b_rg", [4])
    w_re = din("w_re", [D, 32])
    b_re = din("b_re", [32])
    w_gate = din("w_gate", [NE, D, DE])
    w_up = din("w_up", [NE, D, DE])
    w_down = din("w_down", [NE, DE, D])
    norm_f_g = din("norm_f_g", [D])
    consts_d = din("consts", [128, NCONST])
    out_d = nc.dram_tensor("out", [T, D], F32, kind="ExternalOutput").ap()
    mod_d = nc.dram_tensor("mod_s", [6 * D], F32).ap()
    x1_d = nc.dram_tensor("x1_s", [T, D], F32).ap()
    yT_d = nc.dram_tensor("yT_s", [16, 128, T], BF16).ap()
    xs_d = nc.dram_tensor("xs_s", [NE * 2048, D], BF16).ap()
    ys_a = nc.dram_tensor("ys_a", [NE * 2048, D // 2], F32).ap()
    ys_b = nc.dram_tensor("ys_b", [NE * 2048, D // 2], F32).ap()
    r_xs = Res()
    r_mod = Res()
    r_x1 = [Res() for _ in range(NT)]
    r_yT = Res()
    r_ys = Res()

    with contextlib.ExitStack() as es:
        es.enter_context(nc.allow_non_contiguous_dma(reason="small parameter / layout loads"))

        def sb(name, shape, dt=F32):
            return es.enter_context(nc.sbuf_tensor(name, list(shape), dt))

        cf = sb("cf", [128, NCONST], F32)
        cb = sb("cb", [128, NCONST], BF16)
        r_c = Res()

        def CF(k):
            o, n = COFF[k]
            return cf[:, o:o + n]

        def CB(k):
            o, n = COFF[k]
            return cb[:, o:o + n]

        eps_col = CF("misc")[:, 0:1]
        one_col = CF("misc")[:, 1:2]

        R1 = sb("R1", [128, 16, 2048], BF16)
        R2 = sb("R2", [128, 16, 16, 128], BF16)
        R3 = sb("R3", [128, 28672], BF16)
        psum = es.enter_context(nc.psum_tensor("psum", [128, 8, 512], F32))
        small = sb("small", [128, 1024], F32)
        r_small = Res()

        sems = {}
        for e in ("pe", "act", "dve", "pool"):
            sems[e] = es.enter_context(nc.semaphore("s_" + e))
        for l in P.dma_sems.values():
            for s in l:
                sems[s] = es.enter_context(nc.semaphore("s_" + s))

        class Arena:
            def __init__(self, ap2d):
                self.ap = ap2d
                self.off = 0

            def get(self, shape, dt=F32):
                n = int(np.prod(shape[1:]))
                nb = n * (4 if dt in (F32, I32) else 2)
                nel = nb // 2
                a = self.ap[:, self.off:self.off + nel]
                self.off += nel
                assert self.off <= self.ap.shape[1], "arena overflow"
                if dt != BF16:
                    a = a.bitcast(dt)
                if len(shape) == 3:
                    a = a.rearrange("p (a b) -> p a b", b=shape[2])
                elif len(shape) == 4:
                    a = a.rearrange("p (a b c) -> p a b c", b=shape[2], c=shape[3])
                return a

        r1flat = R1[:, :, :].rearrange("p a b -> p (a b)")

        bank_res = [Res() for _ in range(8)]

        class PsPool:
            def __init__(self, banks, width):
                self.slots = []
                for j in range(512 // width):
                    for b in banks:
                        self.slots.append((psum[:, b, j * width:(j + 1) * width], bank_res[b]))
                self.i = 0

            def get(self):
                s = self.slots[self.i % len(self.slots)]
                self.i += 1
                return s

        def mm(out, lhsT, rhs, start, stop, reads, writes):
            P.op("pe", lambda e: e.matmul(out, lhsT, rhs, start=start, stop=stop), reads, writes)

        def tr(out, in_, ident, reads, writes):
            P.op("pe", lambda e: e.transpose(out, in_, ident), reads, writes)

        def act(out, in_, func, reads, writes, scale=None, bias=None, accum=None):
            kw = {}
            if scale is not None:
                kw["scale"] = scale
            if bias is not None:
                kw["bias"] = bias
            if accum is not None:
                kw["accum_out"] = accum
            P.op("act", lambda e: e.activation(out=out, in_=in_, func=func, **kw), reads, writes)

        def ts(eng, out, in0, s1, s2, op0, op1, reads, writes):
            if op1 is None:
                P.op(eng, lambda e: e.tensor_scalar(out=out, in0=in0, scalar1=s1, scalar2=None, op0=op0), reads, writes)
            else:
                P.op(eng, lambda e: e.tensor_scalar(out=out, in0=in0, scalar1=s1, scalar2=s2, op0=op0, op1=op1), reads, writes)

        def tt(eng, out, in0, in1, op, reads, writes):
            P.op(eng, lambda e: e.tensor_tensor(out=out, in0=in0, in1=in1, op=op), reads, writes)

        def stt(eng, out, in0, scalar, in1, op0, op1, reads, writes):
            P.op(eng, lambda e: e.scalar_tensor_tensor(out=out, in0=in0, scalar=scalar, in1=in1, op0=op0, op1=op1), reads, writes)

        def cp(eng, out, in_, reads, writes):
            if eng == "act":
                P.op("act", lambda e: e.activation(out=out, in_=in_, func=AF.Copy), reads, writes)
            else:
                P.op(eng, lambda e: e.tensor_copy(out=out, in_=in_), reads, writes)

        def memset(eng, ap, val, writes):
            P.op(eng, lambda e: e.memset(ap, val), (), writes)

        def dma(eng, out, in_, reads, writes):
            P.dma(eng, lambda e: e.dma_start(out=out, in_=in_), reads, writes)

        def rsqrt_act(out, in_, scale, reads, writes, tmp, r_tmp):
            act(tmp, in_, AF.Ln, list(reads) + [r_c], [r_tmp], scale=scale, bias=eps_col)
            act(out, tmp, AF.Exp, [r_tmp], writes, scale=-0.5)

        dma("sp", cf[:, :], consts_d, (), [r_c])
        cp("dve", cb[:, :], cf[:, :], [r_c], [r_c])
        ident_f = CF("ident")
        ident_b = CB("ident")
        ones_f = CF("ones")

        so = [0]

        def scol(n):
            a = small[:, so[0]:so[0] + n]
            so[0] += n
            return a

        cT = scol(16)
        cact = sb("cact", [128, 16], BF16)
        modc = scol(96).rearrange("p (k c) -> p k c", c=16)
        badac = scol(96).rearrange("p (k c) -> p k c", c=16)
        g1c = scol(16)
        g2c = scol(16)
        a1c = scol(16)
        b1c = scol(16)
        a2c = scol(16)
        b2c = scol(16)
        ss_col = scol(4)
        cw = scol(96).rearrange("p (a j) -> p a j", j=4)
        pscale = scol(8)
        ogain = scol(1)
        alog_b = scol(8)
        dtb_b = scol(8)
        nexpa_b = scol(8)
        ba = scol(256).rearrange("p (t n) -> p t n", n=16)
        beta_all = scol(128).rearrange("p (t n) -> p t n", n=8)
        g_all = scol(128).rearrange("p (t n) -> p t n", n=8)
        br_b = scol(36)
        assert so[0] <= 1024, so[0]

        dma("sp", cT, c_d, (), [r_small])
        act(cact[:, :], cT, AF.Silu, [r_small], [r_small])
        ar = Arena(R3[:, :])
        wa = [ar.get([128, 16, 512], BF16) for _ in range(3)]
        r_wa = [Res() for _ in range(3)]
        mrow = [ar.get([128, 512], F32) for _ in range(2)]
        r_mrow = [Res(), Res()]
        pp0 = PsPool([0, 1], 512)
        for blk in range(24):
            s = blk % 3
            dma("pool", wa[s], w_ada[:, blk * 512:(blk + 1) * 512].rearrange("(p c) n -> p c n", c=16), (), [r_wa[s]])
            ps, rp = pp0.get()
            for c in range(16):
                mm(ps[0:1, :], cact[:, c:c + 1], wa[s][:, c, :], c == 0, c == 15, [r_small, r_wa[s]], [rp])
            m = blk % 2
            cp("act", mrow[m][0:1, :], ps[0:1, :], [rp], [r_mrow[m]])
            dma("sp", mod_d[blk * 512:(blk + 1) * 512].rearrange("(a n) -> a n", a=1), mrow[m][0:1, :], [r_mrow[m]], [r_mod])
        dma("sp", modc, mod_d.rearrange("(k p c) -> p k c", k=6, c=16), [r_mod], [r_small])
        dma("sp", badac, b_ada.rearrange("(k p c) -> p k c", k=6, c=16), (), [r_small])
        dma("sp", g1c, norm1_g.rearrange("(p c) -> p c", c=16), (), [r_small])
        dma("sp", g2c, norm2_g.rearrange("(p c) -> p c", c=16), (), [r_small])
        rs = [r_small]
        tt("dve", modc, modc, badac, ALU.add, rs, rs)
        stt("dve", a1c, modc[:, 1, :], 1.0, g1c, ALU.add, ALU.mult, rs, rs)
        cp("dve", b1c, modc[:, 0, :], rs, rs)
        stt("dve", a2c, modc[:, 4, :], 1.0, g2c, ALU.add, ALU.mult, rs, rs)
        cp("dve", b2c, modc[:, 3, :], rs, rs)
        P.barrier()

        if stage == 0:
            ar = Arena(R3[:, :])
            dbg = ar.get([128, 2048], F32)
            r_dbg = Res()
            memset("dve", dbg, 0.0, [r_dbg])
            cp("dve", dbg[:, 0:96], small[:, 16:112], [r_small, r_dbg], [r_dbg])
            cp("dve", dbg[:, 96:160], small[:, 240:304], [r_small, r_dbg], [r_dbg])
            dma("sp", out_d[0:128, :], dbg, [r_dbg], ())
            P.emit(nc, es.enter_context(nc.Block()), sems)
            return nc
        hT = R1
        r_hT = Res()
        ar = Arena(R3[:, :])
        xb = [ar.get([128, 2048], F32) for _ in range(2)]
        r_xb = [Res(), Res()]
        xn = ar.get([128, 2048], F32)
        r_xn = Res()
        junk = ar.get([128, 2048], BF16)
        r_junk = Res()
        ppA = PsPool([0, 1, 2, 3, 4, 5, 6, 7], 128)
        r_ss = Res()
        for tti in range(NT):
            b = tti % 2
            dma("sp", xb[b], x_d[tti * 128:(tti + 1) * 128, :], (), [r_xb[b]])
            act(junk, xb[b], AF.Square, [r_xb[b]], [r_junk, r_ss], accum=ss_col[:, 0:1])
            rsqrt_act(ss_col[:, 2:3], ss_col[:, 0:1], 1.0 / D, [r_ss], [r_ss], ss_col[:, 1:2], r_ss)
            ts("dve", xn, xb[b], ss_col[:, 2:3], None, ALU.mult, None, [r_xb[b], r_ss], [r_xn])
            xnv = xn.rearrange("t (p c) -> t c p", c=16)
            for c in range(16):
                ps, rp = ppA.get()
                tr(ps, xnv[:, c, :], ident_f, [r_xn, r_c], [rp])
                dst = hT[:, c, tti * 128:(tti + 1) * 128]
                if c % 2 == 0:
                    act(dst, ps, AF.Identity, [rp, r_small], [r_hT], scale=a1c[:, c:c + 1], bias=b1c[:, c:c + 1])
                else:
                    ts("dve", dst, ps, a1c[:, c:c + 1], b1c[:, c:c + 1], ALU.mult, ALU.add, [rp, r_small], [r_hT])
        P.barrier()

        if stage == 1:
            ar = Arena(R3[:, :])
            dbg = ar.get([128, 2048], F32)
            r_dbg = Res()
            for c in range(16):
                cp("dve", dbg, hT[:, c, :], [r_hT], [r_dbg])
                dma("sp", out_d[c * 128:(c + 1) * 128, :], dbg, [r_dbg], ())
            P.emit(nc, es.enter_context(nc.Block()), sems)
            return nc

        ar = Arena(R3[:, :])
        wba = ar.get([128, 16, 16], BF16)
        r_wba = Res()
        dma("pool", wba, w_in[:, 5120:5136].rearrange("(p c) n -> p c n", c=16), (), [r_wba])
        dma("sp", alog_b, a_log.partition_broadcast(128), (), [r_small])
        dma("sp", dtb_b, dt_bias.partition_broadcast(128), (), [r_small])
        dma("sp", ogain, o_norm_g.rearrange("(p a) -> p a", a=1), (), [r_small])
        dma("sp", pscale, pool_scale.rearrange("(j p) -> p j", p=128), (), [r_small])
        cwraw = ar.get([128, 3072], F32)
        r_cwraw = Res()
        dma("sp", cwraw[0:4, :], conv_w, (), [r_cwraw])
        ppw = PsPool([0, 1, 2, 3], 512)
        ps, rp = ppw.get()
        for a in range(24):
            tr(ps[:, a * 4:(a + 1) * 4], cwraw[0:4, a * 128:(a + 1) * 128], ident_f[0:4, 0:4], [r_cwraw, r_c], [rp])
        cp("dve", cw.rearrange("p a j -> p (a j)"), ps[:, 0:96], [rp], [r_small])
        act(nexpa_b, alog_b, AF.Exp, rs, rs)
        ts("dve", nexpa_b, nexpa_b, -1.0, None, ALU.mult, None, rs, rs)
        ppn = PsPool([4, 5, 6, 7], 128)
        for tti in range(NT):
            ps, rp = ppn.get()
            for c in range(16):
                mm(ps[:, 0:16], hT[:, c, tti * 128:(tti + 1) * 128], wba[:, c, :], c == 0, c == 15, [r_hT, r_wba], [rp])
            cp("dve", ba[:, tti, :], ps[:, 0:16], [rp], rs)
            tt("dve", ba[:, tti, 8:16], ba[:, tti, 8:16], dtb_b, ALU.add, rs, rs)
        for tti in range(NT):
            act(beta_all[:, tti, :], ba[:, tti, 0:8], AF.Sigmoid, rs, rs)
        for tti in range(NT):
            act(ba[:, tti, 8:16], ba[:, tti, 8:16], AF.Exp, rs, rs)
        for tti in range(NT):
            act(ba[:, tti, 8:16], ba[:, tti, 8:16], AF.Ln, rs + [r_c], rs, bias=one_col)
        for tti in range(NT):
            tt("dve", g_all[:, tti, :], ba[:, tti, 8:16], nexpa_b, ALU.mult, rs, rs)
        P.barrier()

        ar = Arena(R3[:, :])
        wq = [ar.get([128, 16, 128], BF16) for _ in range(4)]
        r_wq = [Res() for _ in range(4)]
        pre = [[ar.get([128, 516], F32) for _ in range(2)] for _ in range(3)]
        r_pre = [[Res(), Res()] for _ in range(3)]
        cv = [ar.get([128, 512], F32) for _ in range(3)]
        r_cv = [Res() for _ in range(3)]
        sq = [ar.get([128, 512], F32) for _ in range(2)]
        r_sq = [Res(), Res()]
        rinv = [ar.get([128, 512], F32) for _ in range(2)]
        r_rinv = [Res(), Res()]
        lntmp = ar.get([128, 512], F32)
        r_lntmp = Res()
        zs = ar.get([128, 512], F32)
        r_zs = Res()
        QT = ar.get([128, 512], BF16)
        KT = ar.get([128, 512], BF16)
        VT = ar.get([128, 512], BF16)
        r_QT, r_KT, r_VT = Res(), Res(), Res()
        oT = ar.get([128, 512], F32)
        r_oT = Res()
        osq = ar.get([128, 512], F32)
        r_osq = Res()
        ydn = ar.get([128, 512], BF16)
        r_ydn = Res()
        S = ar.get([128, 128], F32)
        Sb = ar.get([128, 128], BF16)
        r_S, r_Sb = Res(), Res()

        class TileBufs:
            pass

        tb = []
        ar = Arena(R2[:, :, :, :].rearrange("p a b c -> p (a b c)"))
        for i in range(4):
            b = TileBufs()
            b.Gb = ar.get([128, 128], F32)
            b.nGb = ar.get([128, 128], F32)
            b.EGb = ar.get([128, 128], F32)
            b.DT = ar.get([128, 128], F32)
            b.cols = ar.get([128, 4], F32)
            b.Mf = ar.get([128, 128], BF16)
            b.qkd = ar.get([128, 128], BF16)
            b.Lo = [ar.get([128, 128], BF16) for _ in range(7)]
            b.X = [ar.get([128, 128], BF16) for _ in range(2)]
            b.XT = [ar.get([128, 128], BF16) for _ in range(2)]
            b.ET = ar.get([128, 128], BF16)
            b.Kd = ar.get([128, 128], BF16)
            b.Vt = ar.get([128, 128], F32)
            b.QgT = ar.get([128, 128], BF16)
            b.R = ar.get([128, 128], BF16)
            b.vn = ar.get([128, 128], BF16)
            b.r = defaultdict(Res)
            tb.append(b)
        ppw = PsPool([0, 1, 2, 3], 512)
        ppn = PsPool([4, 5, 6, 7], 128)
        lmask = [CB("lm%d" % l) for l in range(1, 8)]

        def psb(ps):
            return ps[:, 0:64].bitcast(BF16)

        def prep_tile(b, pb, i, tti, hd):
            r = b.r
            sl = slice(i * 128, (i + 1) * 128)
            gcol = g_all[:, tti, hd:hd + 1]
            bcol = beta_all[:, tti, hd:hd + 1]
            ts("pool", b.Gb, ones_f, gcol, None, ALU.mult, None, [r_c, r_small], [r["Gb"]])
            ts("pool", b.nGb, b.Gb, -1.0, None, ALU.mult, None, [r["Gb"]], [r["nGb"]])
            yield
            psA, rA = ppn.get()
            mm(psA, b.Gb, CF("uinc"), True, True, [r["Gb"], r_c], [rA])
            act(b.EGb, psA, AF.Exp, [rA], [r["EGb"]])
            yield
            psB, rB = ppn.get()
            mm(psB, b.Gb, CF("uinc"), True, False, [r["Gb"], r_c], [rB])
            mm(psB, CF("uinc"), b.nGb, False, False, [r["nGb"], r_c], [rB])
            mm(psB, ident_f, CF("mneg"), False, True, [r_c], [rB])
            act(b.DT, psB, AF.Exp, [rB], [r["DT"]])
            yield
            psg, rg = ppn.get()
            mm(psg[:, 0:1], CF("uinc"), gcol, True, True, [r_c, r_small], [rg])
            act(b.cols[:, 0:1], psg[:, 0:1], AF.Exp, [rg], [r["cols"]])
            ts("dve", b.cols[:, 1:2], b.cols[:, 0:1], -1.0, None, ALU.mult, None, [r["cols"]], [r["cols"]])
            yield
            psK, rK = ppn.get()
            mm(psK, KT[:, sl], KT[:, sl], True, True, [r_KT], [rK])
            stt("dve", b.Mf, psK, bcol, b.DT, ALU.mult, ALU.mult, [rK, r_small, r["DT"]], [r["Mf"]])
            yield
            psQ, rQ = ppn.get()
            mm(psQ, KT[:, sl], QT[:, sl], True, True, [r_KT, r_QT], [rQ])
            tt("dve", b.qkd, psQ, b.DT, ALU.mult, [rQ, r["DT"]], [r["qkd"]])
            yield
            for l in range(7):
                tt("pool", b.Lo[l], b.Mf, lmask[l], ALU.mult, [r["Mf"], r_c], [r["Lo%d" % l]])
            tt("dve", b.X[0], ident_b, b.Lo[0], ALU.subtract, [r_c, r["Lo0"]], [r["X0"]])
            yield
            psT, rT = ppn.get()
            tr(psb(psT), b.X[0], ident_b, [r["X0"], r_c], [rT])
            cp("act", b.XT[0], psb(psT), [rT], [r["XT0"]])
            yield
            cur = 0
            for l in range(1, 7):
                nxt = 1 - cur
                psE, rE = ppn.get()
                mm(psE, b.Lo[l], b.XT[cur], True, True, [r["Lo%d" % l], r["XT%d" % cur]], [rE])
                cp("act", b.ET, psE, [rE], [r["ET"]])
                yield
                psX, rX = ppn.get()
                mm(psX, b.ET, b.X[cur], True, True, [r["ET"], r["X%d" % cur]], [rX])
                tt("dve", b.X[nxt], b.X[cur], psX, ALU.subtract, [r["X%d" % cur], rX], [r["X%d" % nxt]])
                if l < 6:
                    psY, rY = ppn.get()
                    mm(psY, b.X[cur], b.ET, True, True, [r["ET"], r["X%d" % cur]], [rY])
                    tt("dve", b.XT[nxt], b.XT[cur], psY, ALU.subtract, [r["XT%d" % cur], rY], [r["XT%d" % nxt]])
                cur = nxt
                yield
            b.Xf = b.X[cur]
            b.rXf = r["X%d" % cur]
            psT, rT = ppn.get()
            tr(psb(psT), KT[:, sl], ident_b, [r_KT, r_c], [rT])
            ts("dve", b.Kd, psb(psT), b.DT[:, 127:128], None, ALU.mult, None, [rT, r["DT"]], [r["Kd"]])
            yield
            psT, rT = ppn.get()
            tr(psb(psT), VT[:, sl], ident_b, [r_VT, r_c], [rT])
            cp("act", b.Vt, psb(psT), [rT], [r["Vt"]])
            tt("dve", b.QgT, QT[:, sl], b.EGb, ALU.mult, [r_QT, r["EGb"]], [r["QgT"]])
            yield


        for hd in range(8):
            memset("dve", S, 0.0, [r_S])
            memset("dve", Sb, 0.0, [r_Sb])
            for s4 in range(4):
                col0 = 1024 + s4 * 1024 + hd * 128
                dma("pool", wq[s4], w_in[:, col0:col0 + 128].rearrange("(p c) n -> p c n", c=16), (), [r_wq[s4]])
            for tg in range(4):
                t0 = tg * 512
                pp_ = tg % 2
                pss = []
                for s4 in range(4):
                    ps, rp = ppw.get()
                    for c in range(16):
                        mm(ps, wq[s4][:, c, :], hT[:, c, t0:t0 + 512], c == 0, c == 15, [r_wq[s4], r_hT], [rp])
                    pss.append((ps, rp))
                for s3 in range(3):
                    ps, rp = pss[s3]
                    buf = pre[s3][pp_]
                    rb_ = r_pre[s3][pp_]
                    cp("act", buf[:, 4:516], ps, [rp], [rb_])
                    if tg == 0:
                        memset("dve", buf[:, 0:4], 0.0, [rb_])
                    else:
                        cp("dve", buf[:, 0:4], pre[s3][1 - pp_][:, 512:516], [r_pre[s3][1 - pp_]], [rb_])
                    a = s3 * 8 + hd
                    ts("dve", cv[s3], buf[:, 1:513], cw[:, a, 0:1], None, ALU.mult, None, [rb_, r_small], [r_cv[s3]])
                    for j in range(1, 4):
                        stt("dve", cv[s3], buf[:, 1 + j:513 + j], cw[:, a, j:j + 1], cv[s3], ALU.mult, ALU.add, [rb_, r_small], [r_cv[s3]])
                    act(cv[s3], cv[s3], AF.Silu, [r_cv[s3]], [r_cv[s3]])
                ps, rp = pss[3]
                act(zs, ps, AF.Silu, [rp, r_zs], [r_zs])
                for s2 in range(2):
                    act(sq[s2], cv[s2], AF.Square, [r_cv[s2]], [r_sq[s2]])
                    ps, rp = ppw.get()
                    mm(ps, ones_f, sq[s2], True, True, [r_c, r_sq[s2]], [rp])
                    rsqrt_act(rinv[s2], ps, 1.0, [rp], [r_rinv[s2]], lntmp, r_lntmp)
                stt("dve", QT, cv[0], float(128 ** -0.5), rinv[0], ALU.mult, ALU.mult, [r_cv[0], r_rinv[0]], [r_QT])
                tt("dve", KT, cv[1], rinv[1], ALU.mult, [r_cv[1], r_rinv[1]], [r_KT])
                cp("dve", VT, cv[2], [r_cv[2]], [r_VT])
                gens = [prep_tile(tb[i], 0, i, tg * 4 + i, hd) for i in range(4)]
                while gens:
                    for g_ in list(gens):
                        try:
                            next(g_)
                        except StopIteration:
                            gens.remove(g_)
                for i in range(4):
                    b = tb[i]
                    r = b.r
                    tti = tg * 4 + i
                    sl = slice(i * 128, (i + 1) * 128)
                    bcol = beta_all[:, tti, hd:hd + 1]
                    psP, rP = ppn.get()
                    mm(psP, KT[:, sl], Sb, True, True, [r_KT, r_Sb], [rP])
                    stt("dve", b.R, psP, b.cols[:, 1:2], b.Vt, ALU.mult, ALU.add, [rP, r["cols"], r["Vt"]], [r["R"]])
                    psY, rY = ppn.get()
                    mm(psY, b.Xf, b.R, True, True, [b.rXf, r["R"]], [rY])
                    ts("dve", b.vn, psY, bcol, None, ALU.mult, None, [rY, r_small], [r["vn"]])
                    psO, rO = ppn.get()
                    mm(psO, Sb, b.QgT, True, False, [r_Sb, r["QgT"]], [rO])
                    mm(psO, b.vn, b.qkd, False, True, [r["vn"], r["qkd"]], [rO])
                    cp("act", oT[:, sl], psO, [rO], [r_oT])
                    psD, rD = ppn.get()
                    mm(psD, b.Kd, b.vn, True, True, [r["Kd"], r["vn"]], [rD])
                    stt("dve", S, S, b.EGb[:, 127:128], psD, ALU.mult, ALU.add, [r_S, r["EGb"], rD], [r_S])
                    cp("act", Sb, S, [r_S], [r_Sb])
                act(osq, oT, AF.Square, [r_oT], [r_osq])
                ps, rp = ppw.get()
                mm(ps, ones_f, osq, True, True, [r_c, r_osq], [rp])
                rsqrt_act(osq, ps, 1.0 / 128, [rp], [r_osq], lntmp, r_lntmp)
                tt("dve", oT, oT, osq, ALU.mult, [r_oT, r_osq], [r_oT])
                stt("dve", ydn, zs, ogain[:, 0:1], oT, ALU.mult, ALU.mult, [r_zs, r_small, r_oT], [r_ydn])
                dma("sp", yT_d[8 + hd, :, t0:t0 + 512], ydn, [r_ydn], [r_yT])
        P.barrier()

        ar = Arena(R3[:, :])
        wu = ar.get([128, 16, 256], BF16)
        r_wu = Res()
        pw = ar.get([128, 2, 256], BF16)
        r_pw = Res()
        ub = [[ar.get([128, 528], F32) for _ in range(2)] for _ in range(2)]
        r_ub = [[Res(), Res()], [Res(), Res()]]
        wsA = ar.get([128, 528], F32)
        wsB = ar.get([128, 528], F32)
        r_wsA, r_wsB = Res(), Res()
        dfT = [ar.get([128, 512], BF16) for _ in range(2)]
        r_df = [Res(), Res()]
        t16 = ar.get([128, 16], F32)
        r_t16 = Res()
        ypl = [ar.get([128, 512], BF16) for _ in range(2)]
        r_ypl = [Res(), Res()]
        invc = CF("invc").rearrange("p (g n) -> p g n", n=16)
        for g in range(4):
            wlen = 2 ** (g + 1)
            dma("pool", wu, w_in[:, g * 256:(g + 1) * 256].rearrange("(p c) n -> p c n", c=16), (), [r_wu])
            dma("pool", pw, pool_w[g].rearrange("(cc p) d -> p cc d", p=128), (), [r_pw])
            for tg in range(4):
                t0 = tg * 512
                pp_ = tg % 2
                for cc in range(2):
                    ps, rp = ppw.get()
                    for c in range(16):
                        mm(ps, wu[:, c, cc * 128:(cc + 1) * 128], hT[:, c, t0:t0 + 512], c == 0, c == 15, [r_wu, r_hT], [rp])
                    buf = ub[cc][pp_]
                    rb_ = r_ub[cc][pp_]
                    cp("act", buf[:, 16:528], ps, [rp], [rb_])
                    if tg == 0:
                        memset("dve", buf[:, 0:16], 0.0, [rb_])
                    else:
                        cp("dve", buf[:, 0:16], ub[cc][1 - pp_][:, 512:528], [r_ub[cc][1 - pp_]], [rb_])
                    src, rsrc = buf, rb_
                    k = 1
                    tog = 0
                    while k < wlen:
                        dst, rdst = (wsA, r_wsA) if tog == 0 else (wsB, r_wsB)
                        tt("dve", dst[:, k:528], src[:, k:528], src[:, 0:528 - k], ALU.add, [rsrc], [rdst])
                        if k > 1:
                            pass
                        src, rsrc = dst, rdst
                        k *= 2
                        tog = 1 - tog
                    stt("dve", dfT[cc], src[:, 16:528], float(1.0 / wlen), buf[:, 16:528], ALU.mult, ALU.subtract, [rsrc, rb_], [r_df[cc]])
                    if tg == 0:
                        tt("dve", t16, src[:, 16:32], invc[:, g, :], ALU.mult, [rsrc, r_c], [r_t16])
                        tt("dve", dfT[cc][:, 0:16], t16, buf[:, 16:32], ALU.subtract, [r_t16, rb_], [r_df[cc]])
                for ddc in range(2):
                    ps, rp = ppw.get()
                    for cc in range(2):
                        mm(ps, pw[:, cc, ddc * 128:(ddc + 1) * 128], dfT[cc], cc == 0, cc == 1, [r_pw, r_df[cc]], [rp])
                    j = g * 2 + ddc
                    ts("dve", ypl[ddc], ps, pscale[:, j:j + 1], None, ALU.mult, None, [rp, r_small], [r_ypl[ddc]])
                    dma("sp", yT_d[j, :, t0:t0 + 512], ypl[ddc], [r_ypl[ddc]], [r_yT])
        P.barrier()

        Wout = R1
        r_W = [Res() for _ in range(16)]
        ar = Arena(R3[:, :])
        gate_b = ar.get([128, 2048], F32)
        gtmp = ar.get([128, 2048], F32)
        r_gate = Res()
        r_gtmp = Res()
        dma("sp", gate_b, mod_d[2 * D:3 * D].partition_broadcast(128), [r_mod], [r_gate])
        dma("sp", gtmp, b_ada[2 * D:3 * D].partition_broadcast(128), (), [r_gtmp])
        tt("dve", gate_b, gate_b, gtmp, ALU.add, [r_gate, r_gtmp], [r_gate])
        for c in range(16):
            dma("pool", Wout[:, c, :], w_out[c * 128:(c + 1) * 128, :], (), [r_W[c]])
            tt("pool" if c % 2 else "dve", Wout[:, c, :], Wout[:, c, :], gate_b, ALU.mult, [r_W[c], r_gate], [r_W[c]])
        YH = R2
        r_YH = [Res() for _ in range(NT)]
        for tti in range(NT):
            dma("sp", YH[:, tti, :, :], yT_d[:, :, tti * 128:(tti + 1) * 128].rearrange("c p q -> p c q"), [r_yT], [r_YH[tti]])
        P.barrier()
        ar = Arena(R3[:, :])
        xb = [ar.get([128, 2048], F32) for _ in range(2)]
        r_xb = [Res(), Res()]
        xn = ar.get([128, 2048], F32)
        xn2_ = ar.get([128, 2048], F32)
        r_xn = Res()
        junk = ar.get([128, 2048], BF16)
        r_junk = Res()
        h2T = [ar.get([128, 128], F32) for _ in range(4)]
        r_h2T = [Res() for _ in range(4)]
        wr = ar.get([128, 16, 36], F32)
        r_wr = Res()
        dma("sp", wr[:, :, 0:4], w_rg.rearrange("(p c) n -> p c n", c=16), (), [r_wr])
        dma("sp", wr[:, :, 4:36], w_re.rearrange("(p c) n -> p c n", c=16), (), [r_wr])
        dma("sp", br_b[:, 0:4], b_rg.partition_broadcast(128), (), [r_small])
        dma("sp", br_b[:, 4:36], b_re.partition_broadcast(128), (), [r_small])
        lg = ar.get([128, 36], F32)
        rt = ar.get([128, 64], F32)
        og = ar.get([128, 4], F32)
        lem = ar.get([128, 32], F32)
        oh1 = ar.get([128, 32], F32)
        oh2 = ar.get([128, 32], F32)
        ohs = ar.get([128, 32], F32)
        pos = ar.get([128, 32], F32)
        cnt_b = ar.get([128, 32], F32)
        tmp32 = ar.get([128, 32], F32)
        pos_all = sb("pos_all", [128, 16, 32], F32)
        oh1_all = sb("oh1_all", [128, 16, 32], F32)
        oh2_all = sb("oh2_all", [128, 16, 32], F32)
        destf = sb("destf", [128, 16, 2], F32)
        desti = sb("desti", [128, 16, 2], I32)
        cnt_i = sb("cnt_i", [128, 32], I32)
        destib = sb("destib", [128, 16, 2], I32)
        wts = sb("wts", [128, 16, 2], F32)
        r_rt = Res()
        r_route = Res()
        memset("dve", cnt_b, 0.0, [r_rt])
        ppw = PsPool([0, 1, 2, 3], 512)
        ppn = PsPool([4, 5, 6], 128)
        pplg = PsPool([7], 128)
        xns = [xn, xn2_]
        r_xns = [r_xn, Res()]

        def mixA(tti):
            b = tti % 2
            xn = xns[b]
            r_xn = r_xns[b]
            dma("sp", xb[b], x_d[tti * 128:(tti + 1) * 128, :], (), [r_xb[b]])
            banks = []
            for nb in range(4):
                ps, rp = ppw.get()
                for c in range(16):
                    mm(ps, YH[:, tti, c, :], Wout[:, c, nb * 512:(nb + 1) * 512], c == 0, c == 15, [r_YH[tti], r_W[c]], [rp])
                banks.append((ps, rp))
            for nb in range(4):
                ps, rp = banks[nb]
                tt("dve", xb[b][:, nb * 512:(nb + 1) * 512], ps, xb[b][:, nb * 512:(nb + 1) * 512], ALU.add, [rp, r_xb[b]], [r_xb[b]])
            dma("sp", x1_d[tti * 128:(tti + 1) * 128, :], xb[b], [r_xb[b]], [r_x1[tti]])
            act(junk, xb[b], AF.Square, [r_xb[b]], [r_junk, r_ss], accum=ss_col[:, 0:1])
            rsqrt_act(ss_col[:, 2:3], ss_col[:, 0:1], 1.0 / D, [r_ss], [r_ss], ss_col[:, 1:2], r_ss)
            ts("dve", xn, xb[b], ss_col[:, 2:3], None, ALU.mult, None, [r_xb[b], r_ss], [r_xn])
            cp("pool", YH[:, tti, :, :].rearrange("t c p -> t (c p)"), xn, [r_xn], [r_YH[tti]])

        def postB(tti):
            b = tti % 2
            xn = xns[b]
            r_xn = r_xns[b]
            xnv = xn.rearrange("t (p c) -> t c p", c=16)
            psl, rpl = pplg.get()
            for c in range(16):
                ps, rp = ppn.get()
                tr(ps, xnv[:, c, :], ident_f, [r_xn, r_c], [rp])
                hb = h2T[c % 4]
                rhb = r_h2T[c % 4]
                if c % 2 == 0:
                    act(hb, ps, AF.Identity, [rp, r_small], [rhb], scale=a2c[:, c:c + 1], bias=b2c[:, c:c + 1])
                else:
                    ts("dve", hb, ps, a2c[:, c:c + 1], b2c[:, c:c + 1], ALU.mult, ALU.add, [rp, r_small], [rhb])
                mm(psl[:, 0:36], hb, wr[:, c, :], c == 0, c == 15, [rhb, r_wr], [rpl])
            R_ = [r_rt]
            tt("dve", lg, psl[:, 0:36], br_b, ALU.add, [rpl, r_small, r_rt], R_)
            P.op("dve", lambda e: e.reduce_max(out=rt[:, 0:1], in_=lg[:, 0:4], axis=mybir.AxisListType.X), R_, R_)
            ts("dve", og, lg[:, 0:4], rt[:, 0:1], None, ALU.is_equal, None, R_, R_)
            ts("dve", rt[:, 1:2], rt[:, 0:1], -1.0, None, ALU.mult, None, R_, R_)
            act(rt[:, 4:8], lg[:, 0:4], AF.Exp, R_, R_, bias=rt[:, 1:2], accum=rt[:, 2:3])
            P.op("dve", lambda e: e.reciprocal(out=rt[:, 3:4], in_=rt[:, 2:3]), R_, R_)
            for g in range(4):
                ts("dve", rt[:, 8:9], og[:, g:g + 1], 1.0, 1.0e4, ALU.subtract, ALU.mult, R_, R_)
                ts("dve", lem[:, g * 8:(g + 1) * 8], lg[:, 4 + g * 8:12 + g * 8], rt[:, 8:9], None, ALU.add, None, R_, R_)
            P.op("dve", lambda e: e.reduce_max(out=rt[:, 9:10], in_=lem, axis=mybir.AxisListType.X), R_, R_)
            ts("dve", oh1, lem, rt[:, 9:10], None, ALU.is_equal, None, R_, R_)
            stt("dve", tmp32, oh1, -1.0e4, lem, ALU.mult, ALU.add, R_, R_)
            P.op("dve", lambda e: e.reduce_max(out=rt[:, 10:11], in_=tmp32, axis=mybir.AxisListType.X), R_, R_)
            ts("dve", oh2, tmp32, rt[:, 10:11], None, ALU.is_equal, None, R_, R_)
            tt("dve", ohs, oh1, oh2, ALU.add, R_, R_)
            tt("dve", rt[:, 11:12], rt[:, 10:11], rt[:, 9:10], ALU.subtract, R_, R_)
            act(rt[:, 12:13], rt[:, 11:12], AF.Exp, R_, R_)
            ts("dve", rt[:, 12:13], rt[:, 12:13], 1.0, None, ALU.add, None, R_, R_)
            P.op("dve", lambda e: e.reciprocal(out=rt[:, 13:14], in_=rt[:, 12:13]), R_, R_)
            tt("dve", wts[:, tti, 0:1], rt[:, 13:14], rt[:, 3:4], ALU.mult, R_, R_ + [r_route])
            tt("dve", wts[:, tti, 1:2], rt[:, 3:4], wts[:, tti, 0:1], ALU.subtract, R_ + [r_route], R_ + [r_route])
            psp, rpp = ppn.get()
            mm(psp[:, 0:32], CF("ustr"), ohs, True, True, [r_c, r_rt], [rpp])
            tt("dve", pos_all[:, tti, :], psp[:, 0:32], cnt_b, ALU.add, [rpp] + R_, R_ + [r_route])
            psc, rpc = ppn.get()
            mm(psc[:, 0:32], ones_f, ohs, True, True, [r_c, r_rt], [rpc])
            tt("dve", cnt_b, cnt_b, psc[:, 0:32], ALU.add, [rpc] + R_, R_)
            cp("dve", oh1_all[:, tti, :], oh1, R_, R_ + [r_route])
            cp("dve", oh2_all[:, tti, :], oh2, R_, R_ + [r_route])
        mixA(0)
        for tti in range(NT):
            if tti + 1 < NT:
                mixA(tti + 1)
            postB(tti)
        RR = [r_rt, r_route]
        for tti in range(NT):
            tt("dve", tmp32, pos_all[:, tti, :], CF("eoff"), ALU.add, RR + [r_c], RR)
            tt("dve", pos, tmp32, oh1_all[:, tti, :], ALU.mult, RR, RR)
            P.op("dve", lambda e, tti=tti: e.reduce_sum(out=destf[:, tti, 0:1], in_=pos, axis=mybir.AxisListType.X), RR, RR)
            tt("dve", pos, tmp32, oh2_all[:, tti, :], ALU.mult, RR, RR)
            P.op("dve", lambda e, tti=tti: e.reduce_sum(out=destf[:, tti, 1:2], in_=pos, axis=mybir.AxisListType.X), RR, RR)
        cp("dve", desti[:, :, :].rearrange("p a b -> p (a b)"), destf[:, :, :].rearrange("p a b -> p (a b)"), RR, RR)
        cp("dve", cnt_i[:, :], cnt_b, RR, RR)
        dfl = destf[:, :, :].rearrange("p a b -> p (a b)")
        dfb = ar.get([128, 32], F32)
        ts("dve", dfb, dfl, 32768.0, 100000.0, ALU.is_lt, ALU.mult, RR, RR)
        stt("dve", dfb, dfl, -32768.0, dfb, ALU.add, ALU.add, RR, RR)
        cp("dve", destib[:, :, :].rearrange("p a b -> p (a b)"), dfb, RR, RR)
        P.barrier()

        if stage == 2:
            for tti in range(NT):
                b = tti % 2
                dma("sp", xb[b], x1_d[tti * 128:(tti + 1) * 128, :], [r_x1[tti]], [r_xb[b]])
                dma("sp", out_d[tti * 128:(tti + 1) * 128, :], xb[b], [r_xb[b]], ())
            P.emit(nc, es.enter_context(nc.Block()), sems)
            return nc

        NK = 16
        r2flat = R2[:, :, :, :].rearrange("p a b c -> p (a b c)")
        r3flat = R3[:, :]
        zt = r3flat[:, 0:2048]
        r_zt = Res()
        memset("dve", zt, 0.0, [r_zt])
        for e_ in range(NE):
            for k in range(NK):
                P.begin_cond(cnt_i[0:1, e_:e_ + 1], k * 128 + 1)
                row0 = e_ * 2048 + k * 128
                dma("sp", xs_d[row0:row0 + 128, :], zt, [r_zt], [Res()])
            for k in range(NK):
                P.end_cond()
        P.barrier()
        for tti in range(NT):
            for k in range(2):
                P.dma("pool", lambda e, tti=tti, k=k: e.indirect_dma_start(
                    out=xs_d, out_offset=bass.IndirectOffsetOnAxis(ap=desti[:, tti, k:k + 1], axis=0),
                    in_=YH[:, tti, :, :].rearrange("t c p -> t (c p)"), in_offset=None), [r_YH[tti], r_route], [Res()])
        P.barrier()

        def v3(base, o, n):
            return base[:, o:o + 12288].rearrange("p (c n) -> p c n", n=n)

        def vf(base, o, nel, shape=None, dt=BF16):
            a_ = base[:, o:o + nel]
            if dt != BF16:
                a_ = a_.bitcast(dt)
            if shape is not None:
                a_ = a_.rearrange("p (a b) -> p a b", b=shape)
            return a_

        wsets = [[v3(r1flat, 0, DE), v3(r1flat, 12288, DE), v3(r2flat, 0, D)],
                 [v3(r2flat, 12288, DE), v3(r3flat, 0, DE), v3(r3flat, 12288, D)]]
        r_wset = [[Res() for _ in range(3)] for _ in range(2)]
        yo = [vf(r1flat, 24576, 4096, dt=F32), vf(r1flat, 28672, 4096, dt=F32)]
        r_yo = [Res(), Res()]
        xblk = [vf(r2flat, 24576, 2048), vf(r2flat, 26624, 2048), vf(r3flat, 26624, 2048)]
        r_xblk = [Res(), Res(), Res()]
        XT = [vf(r2flat, 28672, 2048, 128), vf(r2flat, 30720, 2048, 128)]
        r_XT = [[Res() for _ in range(16)] for _ in range(2)]
        Hs = [vf(r3flat, 24576, 256, dt=F32), vf(r3flat, 24832, 256, dt=F32)]
        r_Hs = [Res(), Res()]
        Hh = [vf(r3flat, 25088, 768, 128), vf(r3flat, 25856, 768, 128)]
        r_Hh = [Res(), Res()]
        ppd = PsPool([5, 6, 7], 512)
        srcs = [(w_gate, "(p c) n -> p c n", {"c": 16}), (w_up, "(p c) n -> p c n", {"c": 16}), (w_down, "(c p) n -> p c n", {"p": 128})]
        bi = 0
        gi = 0
        for e_ in range(NE):
            ws = e_ % 2
            for which in range(3):
                wsrc, pat, kw = srcs[which]
                dma("pool", wsets[ws][which], wsrc[e_].rearrange(pat, **kw), (), [r_wset[ws][which]])
            Wg, Wu_, Wd = wsets[ws]
            rWg, rWu, rWd = r_wset[ws]
            for k in range(NK):
                P.begin_cond(cnt_i[0:1, e_:e_ + 1], k * 128 + 1)
                st = bi % 2
                bi += 1
                row0 = e_ * 2048 + k * 128
                sx = (bi - 1) % 3
                xb_ = xblk[sx]
                dma("act", xb_, xs_d[row0:row0 + 128, :], [r_xs], [r_xblk[sx]])
                xv = xb_.rearrange("t (p c) -> t c p", c=16)
                for h in range(2):
                    rp = bank_res[h]
                    for j in range(8):
                        c = h * 8 + j
                        tr(psum[:, h, j * 64:(j + 1) * 64].bitcast(BF16), xv[:, c, :], ident_b, [r_xblk[sx], r_c], [rp])
                    for j in range(8):
                        c = h * 8 + j
                        src_ = psum[:, h, j * 64:(j + 1) * 64].bitcast(BF16)
                        if j % 2 == 0:
                            act(XT[st][:, c, :], src_, AF.Identity, [rp, r_small], [r_XT[st][c]], scale=a2c[:, c:c + 1], bias=b2c[:, c:c + 1])
                        else:
                            ts("dve", XT[st][:, c, :], src_, a2c[:, c:c + 1], b2c[:, c:c + 1], ALU.mult, ALU.add, [rp, r_small], [r_XT[st][c]])
                for hc in range(6):
                    gb = 2 + gi % 3
                    gi += 1
                    rpg = bank_res[gb]
                    psg = psum[:, gb, 0:128]
                    psu = psum[:, gb, 128:256]
                    for c in range(16):
                        mm(psg, Wg[:, c, hc * 128:(hc + 1) * 128], XT[st][:, c, :], c == 0, c == 15, [rWg, r_XT[st][c]], [rpg])
                    for c in range(16):
                        mm(psu, Wu_[:, c, hc * 128:(hc + 1) * 128], XT[st][:, c, :], c == 0, c == 15, [rWu, r_XT[st][c]], [rpg])
                    hb = hc % 2
                    act(Hs[hb], psg, AF.Silu, [rpg], [r_Hs[hb]])
                    tt("dve", Hh[st][:, hc, :], Hs[hb], psu, ALU.mult, [r_Hs[hb], rpg], [r_Hh[st]])
                for nb in range(4):
                    ps, rp = ppd.get()
                    for hc in range(6):
                        mm(ps, Hh[st][:, hc, :], Wd[:, hc, nb * 512:(nb + 1) * 512], hc == 0, hc == 5, [r_Hh[st], rWd], [rp])
                    if nb % 2 == 0:
                        cp("act", yo[st][:, nb * 512:(nb + 1) * 512], ps, [rp], [r_yo[st]])
                    else:
                        cp("dve", yo[st][:, nb * 512:(nb + 1) * 512], ps, [rp], [r_yo[st]])
                dma("sp", ys_a[row0:row0 + 128, :], yo[st][:, 0:1024], [r_yo[st]], [r_ys])
                dma("sp", ys_b[row0:row0 + 128, :], yo[st][:, 1024:2048], [r_yo[st]], [r_ys])
            for k in range(NK):
                P.end_cond()
        P.barrier()

        ar = Arena(R3[:, :])
        g2b = ar.get([128, 2048], F32)
        nfb = ar.get([128, 2048], F32)
        r_g2b = Res()
        r_nfb = Res()
        yas = [ar.get([128, 2048], F32) for _ in range(2)]
        ybufs = [ar.get([128, 2048], F32) for _ in range(2)]
        r_yas = [Res(), Res()]
        r_yb2s = [Res(), Res()]
        ar1 = Arena(r1flat)
        x1t = [ar1.get([128, 2048], F32) for _ in range(2)]
        r_x1t = [Res(), Res()]
        gt2 = ar1.get([128, 2048], F32)
        r_gt2 = Res()
        junk = ar1.get([128, 2048], BF16)
        dma("sp", g2b, mod_d[5 * D:6 * D].partition_broadcast(128), [r_mod], [r_g2b])
        dma("sp", gt2, b_ada[5 * D:6 * D].partition_broadcast(128), (), [r_gt2])
        tt("dve", g2b, g2b, gt2, ALU.add, [r_g2b, r_gt2], [r_g2b])
        dma("sp", nfb, norm_f_g.partition_broadcast(128), (), [r_nfb])
        for tti in range(NT):
            b = tti % 2
            ya, ybuf, r_ya, r_yb2 = yas[b], ybufs[b], r_yas[b], r_yb2s[b]
            for (buf_, rbuf_, k_) in ((ya, r_ya, 0), (ybuf, r_yb2, 1)):
                P.dma("pool", lambda e, tti=tti, buf_=buf_, k_=k_: e.indirect_dma_start(
                    out=buf_[:, 0:1024], out_offset=None, in_=ys_a,
                    in_offset=bass.IndirectOffsetOnAxis(ap=desti[:, tti, k_:k_ + 1], axis=0)), [r_ys, r_route], [rbuf_])
                P.dma("pool", lambda e, tti=tti, buf_=buf_, k_=k_: e.indirect_dma_start(
                    out=buf_[:, 1024:2048], out_offset=None, in_=ys_b,
                    in_offset=bass.IndirectOffsetOnAxis(ap=desti[:, tti, k_:k_ + 1], axis=0)), [r_ys, r_route], [rbuf_])
            dma("sp", x1t[b], x1_d[tti * 128:(tti + 1) * 128, :], [r_x1[tti]], [r_x1t[b]])
            ts("dve", ya, ya, wts[:, tti, 0:1], None, ALU.mult, None, [r_ya, r_route], [r_ya])
            stt("dve", ya, ybuf, wts[:, tti, 1:2], ya, ALU.mult, ALU.add, [r_yb2, r_route, r_ya], [r_ya])
            tt("pool", ya, ya, g2b, ALU.mult, [r_ya, r_g2b], [r_ya])
            tt("dve", x1t[b], x1t[b], ya, ALU.add, [r_x1t[b], r_ya], [r_x1t[b]])
            act(junk, x1t[b], AF.Square, [r_x1t[b]], [r_junk, r_ss], accum=ss_col[:, 0:1])
            rsqrt_act(ss_col[:, 2:3], ss_col[:, 0:1], 1.0 / D, [r_ss], [r_ss], ss_col[:, 1:2], r_ss)
            stt("dve", x1t[b], x1t[b], ss_col[:, 2:3], nfb, ALU.mult, ALU.mult, [r_x1t[b], r_ss, r_nfb], [r_x1t[b]])
            dma("sp", out_d[tti * 128:(tti + 1) * 128, :], x1t[b], [r_x1t[b]], ())

        P.emit(nc, es.enter_context(nc.Block()), sems)
    return nc


_INKEYS = ["w_ada", "b_ada", "norm1_g", "w_in", "pool_w", "pool_scale", "conv_w", "a_log", "dt_bias",
           "o_norm_g", "w_out", "norm2_g", "w_gate", "w_up", "w_down"]


def make_in_maps(inputs, cores):
    shared = {k: np.ascontiguousarray(np.asarray(inputs[k])[0]) for k in _INKEYS}
    shared["w_rg"] = np.ascontiguousarray(np.asarray(inputs["w_router_group"])[0])
    shared["b_rg"] = np.ascontiguousarray(np.asarray(inputs["b_router_group"])[0])
    shared["w_re"] = np.ascontiguousarray(np.asarray(inputs["w_router_expert"])[0])
    shared["b_re"] = np.ascontiguousarray(np.asarray(inputs["b_router_expert"])[0])
    shared["norm_f_g"] = np.ascontiguousarray(np.asarray(inputs["norm_f_g"]))
    shared["consts"] = CONSTS
    x = np.asarray(inputs["x"])
    c = np.asarray(inputs["c"])
    maps = []
    for b in cores:
        m = dict(shared)
        m["x"] = np.ascontiguousarray(x[b])
        m["c"] = np.ascontiguousarray(c[b].reshape(128, 16))
        maps.append(m)
    return maps


def kernel(**inputs):
    nc = build()
    maps = make_in_maps(inputs, list(range(8)))
    res = run_bass_kernel_spmd(nc, maps, core_ids=list(range(8)))
    return np.stack([np.asarray(r["out"], dtype=np.float32) for r in res.results], axis=0)
```

```python
import contextlib
from collections import defaultdict

import numpy as np
import concourse.bass as bass
import concourse.mybir as mybir
from concourse.bass_utils import run_bass_kernel_spmd

F32 = mybir.dt.float32
BF16 = mybir.dt.bfloat16
I32 = mybir.dt.int32
ALU = mybir.AluOpType
AF = mybir.ActivationFunctionType

T = 2048
D = 2048
NT = 16
NE = 32
CAP = 256
DE = 768
EPS = 1e-6
SAME_SYNC = True


class Res:
    __slots__ = ("lw", "rd")

    def __init__(self):
        self.lw = None
        self.rd = {}


class Prog:
    ENGS = ("pe", "act", "dve", "pool", "sp")

    def __init__(self):
        self.q = {e: [] for e in self.ENGS}
        self.cnt = defaultdict(int)
        self.known = {e: defaultdict(int) for e in self.ENGS}
        self.dma_sems = {"sp": ["dsp%d" % i for i in range(8)],
                         "pool": ["dpl%d" % i for i in range(8)],
                         "act": ["dac%d" % i for i in range(4)]}
        self.rr = defaultdict(int)
        self.mult = {}
        for e in ("pe", "act", "dve", "pool"):
            self.mult[e] = 1
        for l in self.dma_sems.values():
            for s in l:
                self.mult[s] = 16

    def _need(self, eng, waits, dep):
        ctr, n = dep
        if ctr == eng and (eng == "pe" or not SAME_SYNC):
            return
        if self.known[eng][ctr] >= n:
            return
        if waits.get(ctr, 0) < n:
            waits[ctr] = n

    def _deps(self, eng, reads, writes):
        waits = {}
        for r in reads:
            if r.lw is not None:
                self._need(eng, waits, r.lw)
        for r in writes:
            if r.lw is not None:
                self._need(eng, waits, r.lw)
            for c, n in r.rd.items():
                self._need(eng, waits, (c, n))
        return waits

    def op(self, eng, fn, reads=(), writes=()):
        waits = self._deps(eng, reads, writes)
        for c, n in waits.items():
            self.known[eng][c] = n
        self.cnt[eng] += 1
        n = self.cnt[eng]
        self.q[eng].append((waits, fn, eng, 1))
        for r in reads:
            if r.rd.get(eng, 0) < n:
                r.rd[eng] = n
        for r in writes:
            r.lw = (eng, n)
            r.rd = {}

    def dma(self, eng, fn, reads=(), writes=()):
        names = self.dma_sems[eng]
        ctr = names[self.rr[eng] % len(names)]
        self.rr[eng] += 1
        waits = self._deps(eng, reads, writes)
        if self.cnt[ctr] > 0:
            self._need(eng, waits, (ctr, self.cnt[ctr]))
        for c, n in waits.items():
            self.known[eng][c] = n
        self.cnt[ctr] += 1
        n = self.cnt[ctr]
        self.q[eng].append((waits, fn, ctr, 16))
        for r in reads:
            r.rd[ctr] = n
        for r in writes:
            r.lw = (ctr, n)
            r.rd = {}

    def barrier(self):
        snap = dict(self.cnt)
        for eng in self.ENGS:
            waits = {}
            for c, n in snap.items():
                if n == 0 or (c == eng and eng == "pe"):
                    continue
                if self.known[eng][c] < n:
                    waits[c] = n
                    self.known[eng][c] = n
            if waits:
                self.q[eng].append((waits, None, None, 0))

    def begin_cond(self, cnt_ap, thr):
        if not hasattr(self, "_cstack"):
            self._cstack = []
        known_snap = {e: dict(k) for e, k in self.known.items()}
        marks = {}
        for eng in self.ENGS:
            m = {"kind": "begin", "ap": cnt_ap, "thr": thr, "pre": dict(self.cnt), "delta": {}}
            marks[eng] = m
            self.q[eng].append((None, m, None, 0))
        self._cstack.append((marks, known_snap))

    def end_cond(self):
        marks, known_snap = self._cstack.pop()
        for eng in self.ENGS:
            m = marks[eng]
            idx = max(i for i, it in enumerate(self.q[eng]) if it[1] is m)
            delta = defaultdict(int)
            mw = {}
            for waits, fn, ctr, inc in self.q[eng][idx + 1:]:
                if ctr is not None and callable(fn):
                    delta[ctr] += 1
                if waits:
                    for c, n in waits.items():
                        n = min(n, m["pre"].get(c, 0))
                        if n > mw.get(c, 0):
                            mw[c] = n
            m["delta"] = dict(delta)
            m["skipwaits"] = mw
            self.q[eng].append((None, {"kind": "end", "begin": m}, None, 0))
        for e in self.ENGS:
            self.known[e] = defaultdict(int, known_snap[e])

    def emit(self, nc, block, sems):
        attr = {"pe": "tensor", "act": "scalar", "dve": "vector", "pool": "gpsimd", "sp": "sync"}
        final = dict(self.cnt)
        for eng in self.ENGS:
            items = self.q[eng]

            def body(e, items=items, eng=eng):
                reg = None
                cms = []
                for waits, fn, ctr, inc in items:
                    if isinstance(fn, dict):
                        if fn["kind"] == "begin":
                            if not fn["delta"]:
                                continue
                            if reg is None:
                                reg = e.alloc_register("cnt_" + eng)
                            e.reg_load(reg, fn["ap"])
                            cm = e.If_lt(reg, fn["thr"])
                            cm.__enter__()
                            for c, n in fn.get("skipwaits", {}).items():
                                if n > 0 and c != eng:
                                    e.wait_ge(sems[c], n * self.mult[c])
                            for c, dn in fn["delta"].items():
                                if fn["pre"].get(c, 0) > 0:
                                    e.wait_ge(sems[c], fn["pre"][c] * self.mult[c])
                                e.sem_inc(sems[c], dn * self.mult[c])
                            cm.__exit__(None, None, None)
                            cm = e.Else()
                            cm.__enter__()
                            cms.append(cm)
                        else:
                            if fn["begin"]["delta"]:
                                cms.pop().__exit__(None, None, None)
                        continue
                    for c, n in waits.items():
                        e.wait_ge(sems[c], n * self.mult[c])
                    if fn is not None:
                        fn(e).then_inc(sems[ctr], inc)
                if eng == "sp":
                    for c, n in final.items():
                        if n > 0:
                            e.wait_ge(sems[c], n * self.mult[c])

            getattr(block, attr[eng])(body)


def make_consts():
    i = np.arange(128)
    cs = {}
    cs["ident"] = np.eye(128, dtype=np.float32)
    cs["uinc"] = (i[:, None] <= i[None, :]).astype(np.float32)
    cs["ustr"] = (i[:, None] < i[None, :]).astype(np.float32)
    cs["mneg"] = np.where(i[None, :] >= i[:, None], 0.0, -30000.0).astype(np.float32)
    cs["ones"] = np.ones((128, 128), np.float32)
    for l in range(1, 8):
        same = (i[:, None] >> l) == (i[None, :] >> l)
        diff = (i[:, None] >> (l - 1)) != (i[None, :] >> (l - 1))
        cs["lm%d" % l] = (same & diff & (i[:, None] < i[None, :])).astype(np.float32)
    cs["iota"] = np.broadcast_to(np.arange(CAP, dtype=np.float32)[None, :], (128, CAP)).copy()
    invc = np.zeros((128, 4, 16), np.float32)
    for g, w in enumerate((2, 4, 8, 16)):
        invc[:, g, :] = 1.0 / np.minimum(np.arange(16) + 1, w)
    cs["invc"] = invc.reshape(128, 64)
    cs["eoff"] = np.broadcast_to((np.arange(NE, dtype=np.float32) * 2048.0)[None, :], (128, NE)).copy()
    cs["b128"] = np.broadcast_to((np.arange(64, dtype=np.float32) * 128.0)[None, :], (128, 64)).copy()
    cs["misc"] = np.broadcast_to(np.array([EPS, 1.0, 0.0, -1.0], np.float32)[None, :], (128, 4)).copy()
    offs = {}
    cols = []
    o = 0
    for k, v in cs.items():
        offs[k] = (o, v.shape[1])
        cols.append(v)
        o += v.shape[1]
    return np.concatenate(cols, axis=1), offs


CONSTS, COFF = make_consts()
NCONST = CONSTS.shape[1]


def build(stage=99):
    nc = bass.Bass("TRN2", target_bir_lowering=False)
    P = Prog()

    def din(name, shape, dt=F32):
        return nc.dram_tensor(name, list(shape), dt, kind="ExternalInput").ap()

    x_d = din("x", [T, D])
    c_d = din("c", [128, 16])
    w_ada = din("w_ada", [D, 6 * D])
    b_ada = din("b_ada", [6 * D])
    norm1_g = din("norm1_g", [D])
    w_in = din("w_in", [D, 5136])
    pool_w = din("pool_w", [4, 256, 256])
    pool_scale = din("pool_scale", [1024])
    conv_w = din("conv_w", [4, 3072])
    a_log = din("a_log", [8])
    dt_bias = din("dt_bias", [8])
    o_norm_g = din("o_norm_g", [128])
    w_out = din("w_out", [D, D])
    norm2_g = din("norm2_g", [D])
    w_rg = din("w_rg", [D, 4])
    b_rg = din("b_rg", [4])
    w_re = din("w_re", [D, 32])
    b_re = din("b_re", [32])
    w_gate = din("w_gate", [NE, D, DE])
    w_up = din("w_up", [NE, D, DE])
    w_down = din("w_down", [NE, DE, D])
    norm_f_g = din("norm_f_g", [D])
    consts_d = din("consts", [128, NCONST])
    out_d = nc.dram_tensor("out", [T, D], F32, kind="ExternalOutput").ap()
    mod_d = nc.dram_tensor("mod_s", [6 * D], F32).ap()
    x1_d = nc.dram_tensor("x1_s", [T, D], F32).ap()
    yT_d = nc.dram_tensor("yT_s", [16, 128, T], BF16).ap()
    xs_d = nc.dram_tensor("xs_s", [NE * 2048, D], BF16).ap()
    ys_a = nc.dram_tensor("ys_a", [NE * 2048, D // 2], F32).ap()
    ys_b = nc.dram_tensor("ys_b", [NE * 2048, D // 2], F32).ap()
    r_xs = Res()
    r_mod = Res()
    r_x1 = [Res() for _ in range(NT)]
    r_yT = Res()
    r_ys = Res()

    with contextlib.ExitStack() as es:
        es.enter_context(nc.allow_non_contiguous_dma(reason="small parameter / layout loads"))

        def sb(name, shape, dt=F32):
            return es.enter_context(nc.sbuf_tensor(name, list(shape), dt))

        cf = sb("cf", [128, NCONST], F32)
        cb = sb("cb", [128, NCONST], BF16)
        r_c = Res()

        def CF(k):
            o, n = COFF[k]
            return cf[:, o:o + n]

        def CB(k):
            o, n = COFF[k]
            return cb[:, o:o + n]

        eps_col = CF("misc")[:, 0:1]
        one_col = CF("misc")[:, 1:2]

        R1 = sb("R1", [128, 16, 2048], BF16)
        R2 = sb("R2", [128, 16, 16, 128], BF16)
        R3 = sb("R3", [128, 28672], BF16)
        psum = es.enter_context(nc.psum_tensor("psum", [128, 8, 512], F32))
        small = sb("small", [128, 1024], F32)
        r_small = Res()

        sems = {}
        for e in ("pe", "act", "dve", "pool"):
            sems[e] = es.enter_context(nc.semaphore("s_" + e))
        for l in P.dma_sems.values():
            for s in l:
                sems[s] = es.enter_context(nc.semaphore("s_" + s))

        class Arena:
            def __init__(self, ap2d):
                self.ap = ap2d
                self.off = 0

            def get(self, shape, dt=F32):
                n = int(np.prod(shape[1:]))
                nb = n * (4 if dt in (F32, I32) else 2)
                nel = nb // 2
                a = self.ap[:, self.off:self.off + nel]
                self.off += nel
                assert self.off <= self.ap.shape[1], "arena overflow"
                if dt != BF16:
                    a = a.bitcast(dt)
                if len(shape) == 3:
                    a = a.rearrange("p (a b) -> p a b", b=shape[2])
                elif len(shape) == 4:
                    a = a.rearrange("p (a b c) -> p a b c", b=shape[2], c=shape[3])
                return a

        r1flat = R1[:, :, :].rearrange("p a b -> p (a b)")

        bank_res = [Res() for _ in range(8)]

        class PsPool:
            def __init__(self, banks, width):
                self.slots = []
                for j in range(512 // width):
                    for b in banks:
                        self.slots.append((psum[:, b, j * width:(j + 1) * width], bank_res[b]))
                self.i = 0

            def get(self):
                s = self.slots[self.i % len(self.slots)]
                self.i += 1
                return s

        def mm(out, lhsT, rhs, start, stop, reads, writes):
            P.op("pe", lambda e: e.matmul(out, lhsT, rhs, start=start, stop=stop), reads, writes)

        def tr(out, in_, ident, reads, writes):
            P.op("pe", lambda e: e.transpose(out, in_, ident), reads, writes)

        def act(out, in_, func, reads, writes, scale=None, bias=None, accum=None):
            kw = {}
            if scale is not None:
                kw["scale"] = scale
            if bias is not None:
                kw["bias"] = bias
            if accum is not None:
                kw["accum_out"] = accum
            P.op("act", lambda e: e.activation(out=out, in_=in_, func=func, **kw), reads, writes)

        def ts(eng, out, in0, s1, s2, op0, op1, reads, writes):
            if op1 is None:
                P.op(eng, lambda e: e.tensor_scalar(out=out, in0=in0, scalar1=s1, scalar2=None, op0=op0), reads, writes)
            else:
                P.op(eng, lambda e: e.tensor_scalar(out=out, in0=in0, scalar1=s1, scalar2=s2, op0=op0, op1=op1), reads, writes)

        def tt(eng, out, in0, in1, op, reads, writes):
            P.op(eng, lambda e: e.tensor_tensor(out=out, in0=in0, in1=in1, op=op), reads, writes)

        def stt(eng, out, in0, scalar, in1, op0, op1, reads, writes):
            P.op(eng, lambda e: e.scalar_tensor_tensor(out=out, in0=in0, scalar=scalar, in1=in1, op0=op0, op1=op1), reads, writes)

        def cp(eng, out, in_, reads, writes):
            if eng == "act":
                P.op("act", lambda e: e.activation(out=out, in_=in_, func=AF.Copy), reads, writes)
            else:
                P.op(eng, lambda e: e.tensor_copy(out=out, in_=in_), reads, writes)

        def memset(eng, ap, val, writes):
            P.op(eng, lambda e: e.memset(ap, val), (), writes)

        def dma(eng, out, in_, reads, writes):
            P.dma(eng, lambda e: e.dma_start(out=out, in_=in_), reads, writes)

        def rsqrt_act(out, in_, scale, reads, writes, tmp, r_tmp):
            act(tmp, in_, AF.Ln, list(reads) + [r_c], [r_tmp], scale=scale, bias=eps_col)
            act(out, tmp, AF.Exp, [r_tmp], writes, scale=-0.5)

        dma("sp", cf[:, :], consts_d, (), [r_c])
        cp("dve", cb[:, :], cf[:, :], [r_c], [r_c])
        ident_f = CF("ident")
        ident_b = CB("ident")
        ones_f = CF("ones")

        so = [0]

        def scol(n):
            a = small[:, so[0]:so[0] + n]
            so[0] += n
            return a

        cT = scol(16)
        cact = sb("cact", [128, 16], BF16)
        modc = scol(96).rearrange("p (k c) -> p k c", c=16)
        badac = scol(96).rearrange("p (k c) -> p k c", c=16)
        g1c = scol(16)
        g2c = scol(16)
        a1c = scol(16)
        b1c = scol(16)
        a2c = scol(16)
        b2c = scol(16)
        ss_col = scol(4)
        cw = scol(96).rearrange("p (a j) -> p a j", j=4)
        pscale = scol(8)
        ogain = scol(1)
        alog_b = scol(8)
        dtb_b = scol(8)
        nexpa_b = scol(8)
        ba = scol(256).rearrange("p (t n) -> p t n", n=16)
        beta_all = scol(128).rearrange("p (t n) -> p t n", n=8)
        g_all = scol(128).rearrange("p (t n) -> p t n", n=8)
        br_b = scol(36)
        assert so[0] <= 1024, so[0]

        dma("sp", cT, c_d, (), [r_small])
        act(cact[:, :], cT, AF.Silu, [r_small], [r_small])
        ar = Arena(R3[:, :])
        wa = [ar.get([128, 16, 512], BF16) for _ in range(3)]
        r_wa = [Res() for _ in range(3)]
        mrow = [ar.get([128, 512], F32) for _ in range(2)]
        r_mrow = [Res(), Res()]
        pp0 = PsPool([0, 1], 512)
        for blk in range(24):
            s = blk % 3
            dma("pool", wa[s], w_ada[:, blk * 512:(blk + 1) * 512].rearrange("(p c) n -> p c n", c=16), (), [r_wa[s]])
            ps, rp = pp0.get()
            for c in range(16):
                mm(ps[0:1, :], cact[:, c:c + 1], wa[s][:, c, :], c == 0, c == 15, [r_small, r_wa[s]], [rp])
            m = blk % 2
            cp("act", mrow[m][0:1, :], ps[0:1, :], [rp], [r_mrow[m]])
            dma("sp", mod_d[blk * 512:(blk + 1) * 512].rearrange("(a n) -> a n", a=1), mrow[m][0:1, :], [r_mrow[m]], [r_mod])
        dma("sp", modc, mod_d.rearrange("(k p c) -> p k c", k=6, c=16), [r_mod], [r_small])
        dma("sp", badac, b_ada.rearrange("(k p c) -> p k c", k=6, c=16), (), [r_small])
        dma("sp", g1c, norm1_g.rearrange("(p c) -> p c", c=16), (), [r_small])
        dma("sp", g2c, norm2_g.rearrange("(p c) -> p c", c=16), (), [r_small])
        rs = [r_small]
        tt("dve", modc, modc, badac, ALU.add, rs, rs)
        stt("dve", a1c, modc[:, 1, :], 1.0, g1c, ALU.add, ALU.mult, rs, rs)
        cp("dve", b1c, modc[:, 0, :], rs, rs)
        stt("dve", a2c, modc[:, 4, :], 1.0, g2c, ALU.add, ALU.mult, rs, rs)
        cp("dve", b2c, modc[:, 3, :], rs, rs)
        P.barrier()

        if stage == 0:
            ar = Arena(R3[:, :])
            dbg = ar.get([128, 2048], F32)
            r_dbg = Res()
            memset("dve", dbg, 0.0, [r_dbg])
            cp("dve", dbg[:, 0:96], small[:, 16:112], [r_small, r_dbg], [r_dbg])
            cp("dve", dbg[:, 96:160], small[:, 240:304], [r_small, r_dbg], [r_dbg])
            dma("sp", out_d[0:128, :], dbg, [r_dbg], ())
            P.emit(nc, es.enter_context(nc.Block()), sems)
            return nc
        hT = R1
        r_hT = Res()
        ar = Arena(R3[:, :])
        xb = [ar.get([128, 2048], F32) for _ in range(2)]
        r_xb = [Res(), Res()]
        xn = ar.get([128, 2048], F32)
        r_xn = Res()
        junk = ar.get([128, 2048], BF16)
        r_junk = Res()
        ppA = PsPool([0, 1, 2, 3, 4, 5, 6, 7], 128)
        r_ss = Res()
        for tti in range(NT):
            b = tti % 2
            dma("sp", xb[b], x_d[tti * 128:(tti + 1) * 128, :], (), [r_xb[b]])
            act(junk, xb[b], AF.Square, [r_xb[b]], [r_junk, r_ss], accum=ss_col[:, 0:1])
            rsqrt_act(ss_col[:, 2:3], ss_col[:, 0:1], 1.0 / D, [r_ss], [r_ss], ss_col[:, 1:2], r_ss)
            ts("dve", xn, xb[b], ss_col[:, 2:3], None, ALU.mult, None, [r_xb[b], r_ss], [r_xn])
            xnv = xn.rearrange("t (p c) -> t c p", c=16)
            for c in range(16):
                ps, rp = ppA.get()
                tr(ps, xnv[:, c, :], ident_f, [r_xn, r_c], [rp])
                dst = hT[:, c, tti * 128:(tti + 1) * 128]
                if c % 2 == 0:
                    act(dst, ps, AF.Identity, [rp, r_small], [Res()], scale=a1c[:, c:c + 1], bias=b1c[:, c:c + 1])
                else:
                    ts("dve", dst, ps, a1c[:, c:c + 1], b1c[:, c:c + 1], ALU.mult, ALU.add, [rp, r_small], [Res()])
        P.barrier()

        if stage == 1:
            ar = Arena(R3[:, :])
            dbg = ar.get([128, 2048], F32)
            r_dbg = Res()
            for c in range(16):
                cp("dve", dbg, hT[:, c, :], [r_hT], [r_dbg])
                dma("sp", out_d[c * 128:(c + 1) * 128, :], dbg, [r_dbg], ())
            P.emit(nc, es.enter_context(nc.Block()), sems)
            return nc

        ar = Arena(R3[:, :])
        wba = ar.get([128, 16, 16], BF16)
        r_wba = Res()
        dma("pool", wba, w_in[:, 5120:5136].rearrange("(p c) n -> p c n", c=16), (), [r_wba])
        dma("sp", alog_b, a_log.partition_broadcast(128), (), [r_small])
        dma("sp", dtb_b, dt_bias.partition_broadcast(128), (), [r_small])
        dma("sp", ogain, o_norm_g.rearrange("(p a) -> p a", a=1), (), [r_small])
        dma("sp", pscale, pool_scale.rearrange("(j p) -> p j", p=128), (), [r_small])
        cwraw = ar.get([128, 3072], F32)
        r_cwraw = Res()
        dma("sp", cwraw[0:4, :], conv_w, (), [r_cwraw])
        ppw = PsPool([0, 1, 2, 3], 512)
        ps, rp = ppw.get()
        for a in range(24):
            tr(ps[:, a * 4:(a + 1) * 4], cwraw[0:4, a * 128:(a + 1) * 128], ident_f[0:4, 0:4], [r_cwraw, r_c], [rp])
        cp("dve", cw.rearrange("p a j -> p (a j)"), ps[:, 0:96], [rp], [r_small])
        act(nexpa_b, alog_b, AF.Exp, rs, rs)
        ts("dve", nexpa_b, nexpa_b, -1.0, None, ALU.mult, None, rs, rs)
        ppn = PsPool([4, 5, 6, 7], 128)
        for tti in range(NT):
            ps, rp = ppn.get()
            for c in range(16):
                mm(ps[:, 0:16], hT[:, c, tti * 128:(tti + 1) * 128], wba[:, c, :], c == 0, c == 15, [r_hT, r_wba], [rp])
            cp("dve", ba[:, tti, :], ps[:, 0:16], [rp], rs)
            tt("dve", ba[:, tti, 8:16], ba[:, tti, 8:16], dtb_b, ALU.add, rs, rs)
        for tti in range(NT):
            act(beta_all[:, tti, :], ba[:, tti, 0:8], AF.Sigmoid, rs, rs)
        for tti in range(NT):
            act(ba[:, tti, 8:16], ba[:, tti, 8:16], AF.Exp, rs, rs)
        for tti in range(NT):
            act(ba[:, tti, 8:16], ba[:, tti, 8:16], AF.Ln, rs + [r_c], rs, bias=one_col)
        for tti in range(NT):
            tt("dve", g_all[:, tti, :], ba[:, tti, 8:16], nexpa_b, ALU.mult, rs, rs)
        P.barrier()

        ar = Arena(R3[:, :])
        wq = [ar.get([128, 16, 128], BF16) for _ in range(4)]
        r_wq = [Res() for _ in range(4)]
        pre = [[ar.get([128, 516], F32) for _ in range(2)] for _ in range(3)]
        r_pre = [[Res(), Res()] for _ in range(3)]
        cv = [ar.get([128, 512], F32) for _ in range(3)]
        r_cv = [Res() for _ in range(3)]
        sq = [ar.get([128, 512], F32) for _ in range(2)]
        r_sq = [Res(), Res()]
        rinv = [ar.get([128, 512], F32) for _ in range(2)]
        r_rinv = [Res(), Res()]
        lntmp = ar.get([128, 512], F32)
        r_lntmp = Res()
        zs = ar.get([128, 512], F32)
        r_zs = Res()
        QT = ar.get([128, 512], BF16)
        KT = ar.get([128, 512], BF16)
        VT = ar.get([128, 512], BF16)
        r_QT, r_KT, r_VT = Res(), Res(), Res()
        oT = ar.get([128, 512], F32)
        r_oT = Res()
        osq = ar.get([128, 512], F32)
        r_osq = Res()
        ydn = ar.get([128, 512], BF16)
        r_ydn = Res()
        S = ar.get([128, 128], F32)
        Sb = ar.get([128, 128], BF16)
        r_S, r_Sb = Res(), Res()

        class TileBufs:
            pass

        tb = []
        ar = Arena(R2[:, :, :, :].rearrange("p a b c -> p (a b c)"))
        for i in range(4):
            b = TileBufs()
            b.Gb = ar.get([128, 128], F32)
            b.nGb = ar.get([128, 128], F32)
            b.EGb = ar.get([128, 128], F32)
            b.DT = ar.get([128, 128], F32)
            b.cols = ar.get([128, 4], F32)
            b.Mf = ar.get([128, 128], BF16)
            b.qkd = ar.get([128, 128], BF16)
            b.Lo = [ar.get([128, 128], BF16) for _ in range(7)]
            b.X = [ar.get([128, 128], BF16) for _ in range(2)]
            b.XT = [ar.get([128, 128], BF16) for _ in range(2)]
            b.ET = ar.get([128, 128], BF16)
            b.Kd = ar.get([128, 128], BF16)
            b.Vt = ar.get([128, 128], F32)
            b.QgT = ar.get([128, 128], BF16)
            b.R = ar.get([128, 128], BF16)
            b.vn = ar.get([128, 128], BF16)
            b.r = defaultdict(Res)
            tb.append(b)
        ppw = PsPool([0, 1, 2, 3], 512)
        ppn = PsPool([4, 5, 6, 7], 128)
        lmask = [CB("lm%d" % l) for l in range(1, 8)]

        def psb(ps):
            return ps[:, 0:64].bitcast(BF16)

        def prep_tile(b, pb, i, tti, hd):
            r = b.r
            sl = slice(i * 128, (i + 1) * 128)
            gcol = g_all[:, tti, hd:hd + 1]
            bcol = beta_all[:, tti, hd:hd + 1]
            ts("pool", b.Gb, ones_f, gcol, None, ALU.mult, None, [r_c, r_small], [r["Gb"]])
            ts("pool", b.nGb, b.Gb, -1.0, None, ALU.mult, None, [r["Gb"]], [r["nGb"]])
            yield
            psA, rA = ppn.get()
            mm(psA, b.Gb, CF("uinc"), True, True, [r["Gb"], r_c], [rA])
            act(b.EGb, psA, AF.Exp, [rA], [r["EGb"]])
            yield
            psB, rB = ppn.get()
            mm(psB, b.Gb, CF("uinc"), True, False, [r["Gb"], r_c], [rB])
            mm(psB, CF("uinc"), b.nGb, False, False, [r["nGb"], r_c], [rB])
            mm(psB, ident_f, CF("mneg"), False, True, [r_c], [rB])
            act(b.DT, psB, AF.Exp, [rB], [r["DT"]])
            yield
            psg, rg = ppn.get()
            mm(psg[:, 0:1], CF("uinc"), gcol, True, True, [r_c, r_small], [rg])
            act(b.cols[:, 0:1], psg[:, 0:1], AF.Exp, [rg], [r["cols"]])
            ts("dve", b.cols[:, 1:2], b.cols[:, 0:1], -1.0, None, ALU.mult, None, [r["cols"]], [r["cols"]])
            yield
            psK, rK = ppn.get()
            mm(psK, KT[:, sl], KT[:, sl], True, True, [r_KT], [rK])
            stt("dve", b.Mf, psK, bcol, b.DT, ALU.mult, ALU.mult, [rK, r_small, r["DT"]], [r["Mf"]])
            yield
            psQ, rQ = ppn.get()
            mm(psQ, KT[:, sl], QT[:, sl], True, True, [r_KT, r_QT], [rQ])
            tt("dve", b.qkd, psQ, b.DT, ALU.mult, [rQ, r["DT"]], [r["qkd"]])
            yield
            for l in range(7):
                tt("pool", b.Lo[l], b.Mf, lmask[l], ALU.mult, [r["Mf"], r_c], [r["Lo%d" % l]])
            tt("dve", b.X[0], ident_b, b.Lo[0], ALU.subtract, [r_c, r["Lo0"]], [r["X0"]])
            yield
            psT, rT = ppn.get()
            tr(psb(psT), b.X[0], ident_b, [r["X0"], r_c], [rT])
            cp("act", b.XT[0], psb(psT), [rT], [r["XT0"]])
            yield
            cur = 0
            for l in range(1, 7):
                nxt = 1 - cur
                psE, rE = ppn.get()
                mm(psE, b.Lo[l], b.XT[cur], True, True, [r["Lo%d" % l], r["XT%d" % cur]], [rE])
                cp("act", b.ET, psE, [rE], [r["ET"]])
                yield
                psX, rX = ppn.get()
                mm(psX, b.ET, b.X[cur], True, True, [r["ET"], r["X%d" % cur]], [rX])
                tt("dve", b.X[nxt], b.X[cur], psX, ALU.subtract, [r["X%d" % cur], rX], [r["X%d" % nxt]])
                if l < 6:
                    psY, rY = ppn.get()
                    mm(psY, b.X[cur], b.ET, True, True, [r["ET"], r["X%d" % cur]], [rY])
                    tt("dve", b.XT[nxt], b.XT[cur], psY, ALU.subtract, [r["XT%d" % cur], rY], [r["XT%d" % nxt]])
                cur = nxt
                yield
            b.Xf = b.X[cur]
            b.rXf = r["X%d" % cur]
            psT, rT = ppn.get()
            tr(psb(psT), KT[:, sl], ident_b, [r_KT, r_c], [rT])
            ts("dve", b.Kd, psb(psT), b.DT[:, 127:128], None, ALU.mult, None, [rT, r["DT"]], [r["Kd"]])
            yield
            psT, rT = ppn.get()
            tr(psb(psT), VT[:, sl], ident_b, [r_VT, r_c], [rT])
            cp("act", b.Vt, psb(psT), [rT], [r["Vt"]])
            tt("dve", b.QgT, QT[:, sl], b.EGb, ALU.mult, [r_QT, r["EGb"]], [r["QgT"]])
            yield


        for hd in range(8):
            memset("dve", S, 0.0, [r_S])
            memset("dve", Sb, 0.0, [r_Sb])
            for s4 in range(4):
                col0 = 1024 + s4 * 1024 + hd * 128
                dma("pool", wq[s4], w_in[:, col0:col0 + 128].rearrange("(p c) n -> p c n", c=16), (), [r_wq[s4]])
            for tg in range(4):
                t0 = tg * 512
                pp_ = tg % 2
                pss = []
                for s4 in range(4):
                    ps, rp = ppw.get()
                    for c in range(16):
                        mm(ps, wq[s4][:, c, :], hT[:, c, t0:t0 + 512], c == 0, c == 15, [r_wq[s4], r_hT], [rp])
                    pss.append((ps, rp))
                for s3 in range(3):
                    ps, rp = pss[s3]
                    buf = pre[s3][pp_]
                    rb_ = r_pre[s3][pp_]
                    cp("act", buf[:, 4:516], ps, [rp], [rb_])
                    if tg == 0:
                        memset("dve", buf[:, 0:4], 0.0, [rb_])
                    else:
                        cp("dve", buf[:, 0:4], pre[s3][1 - pp_][:, 512:516], [r_pre[s3][1 - pp_]], [rb_])
                    a = s3 * 8 + hd
                    ts("dve", cv[s3], buf[:, 1:513], cw[:, a, 0:1], None, ALU.mult, None, [rb_, r_small], [r_cv[s3]])
                    for j in range(1, 4):
                        stt("dve", cv[s3], buf[:, 1 + j:513 + j], cw[:, a, j:j + 1], cv[s3], ALU.mult, ALU.add, [rb_, r_small], [r_cv[s3]])
                    act(cv[s3], cv[s3], AF.Silu, [r_cv[s3]], [r_cv[s3]])
                ps, rp = pss[3]
                act(zs, ps, AF.Silu, [rp, r_zs], [r_zs])
                for s2 in range(2):
                    act(sq[s2], cv[s2], AF.Square, [r_cv[s2]], [r_sq[s2]])
                    ps, rp = ppw.get()
                    mm(ps, ones_f, sq[s2], True, True, [r_c, r_sq[s2]], [rp])
                    rsqrt_act(rinv[s2], ps, 1.0, [rp], [r_rinv[s2]], lntmp, r_lntmp)
                stt("dve", QT, cv[0], float(128 ** -0.5), rinv[0], ALU.mult, ALU.mult, [r_cv[0], r_rinv[0]], [r_QT])
                tt("dve", KT, cv[1], rinv[1], ALU.mult, [r_cv[1], r_rinv[1]], [r_KT])
                cp("dve", VT, cv[2], [r_cv[2]], [r_VT])
                gens = [prep_tile(tb[i], 0, i, tg * 4 + i, hd) for i in range(4)]
                while gens:
                    for g_ in list(gens):
                        try:
                            next(g_)
                        except StopIteration:
                            gens.remove(g_)
                for i in range(4):
                    b = tb[i]
                    r = b.r
                    tti = tg * 4 + i
                    sl = slice(i * 128, (i + 1) * 128)
                    bcol = beta_all[:, tti, hd:hd + 1]
                    psP, rP = ppn.get()
                    mm(psP, KT[:, sl], Sb, True, True, [r_KT, r_Sb], [rP])
                    stt("dve", b.R, psP, b.cols[:, 1:2], b.Vt, ALU.mult, ALU.add, [rP, r["cols"], r["Vt"]], [r["R"]])
                    psY, rY = ppn.get()
                    mm(psY, b.Xf, b.R, True, True, [b.rXf, r["R"]], [rY])
                    ts("dve", b.vn, psY, bcol, None, ALU.mult, None, [rY, r_small], [r["vn"]])
                    psO, rO = ppn.get()
                    mm(psO, Sb, b.QgT, True, False, [r_Sb, r["QgT"]], [rO])
                    mm(psO, b.vn, b.qkd, False, True, [r["vn"], r["qkd"]], [rO])
                    cp("act", oT[:, sl], psO, [rO], [r_oT])
                    psD, rD = ppn.get()
                    mm(psD, b.Kd, b.vn, True, True, [r["Kd"], r["vn"]], [rD])
                    stt("dve", S, S, b.EGb[:, 127:128], psD, ALU.mult, ALU.add, [r_S, r["EGb"], rD], [r_S])
                    cp("act", Sb, S, [r_S], [r_Sb])
                act(osq, oT, AF.Square, [r_oT], [r_osq])
                ps, rp = ppw.get()
                mm(ps, ones_f, osq, True, True, [r_c, r_osq], [rp])
                rsqrt_act(osq, ps, 1.0 / 128, [rp], [r_osq], lntmp, r_lntmp)
                tt("dve", oT, oT, osq, ALU.mult, [r_oT, r_osq], [r_oT])
                stt("dve", ydn, zs, ogain[:, 0:1], oT, ALU.mult, ALU.mult, [r_zs, r_small, r_oT], [r_ydn])
                dma("sp", yT_d[8 + hd, :, t0:t0 + 512], ydn, [r_ydn], [r_yT])
        P.barrier()

        ar = Arena(R3[:, :])
        wu = ar.get([128, 16, 256], BF16)
        r_wu = Res()
        pw = ar.get([128, 2, 256], BF16)
        r_pw = Res()
        ub = [[ar.get([128, 528], F32) for _ in range(2)] for _ in range(2)]
        r_ub = [[Res(), Res()], [Res(), Res()]]
        wsA = ar.get([128, 528], F32)
        wsB = ar.get([128, 528], F32)
        r_wsA, r_wsB = Res(), Res()
        dfT = [ar.get([128, 512], BF16) for _ in range(2)]
        r_df = [Res(), Res()]
        t16 = ar.get([128, 16], F32)
        r_t16 = Res()
        ypl = [ar.get([128, 512], BF16) for _ in range(2)]
        r_ypl = [Res(), Res()]
        invc = CF("invc").rearrange("p (g n) -> p g n", n=16)
        for g in range(4):
            wlen = 2 ** (g + 1)
            dma("pool", wu, w_in[:, g * 256:(g + 1) * 256].rearrange("(p c) n -> p c n", c=16), (), [r_wu])
            dma("pool", pw, pool_w[g].rearrange("(cc p) d -> p cc d", p=128), (), [r_pw])
            for tg in range(4):
                t0 = tg * 512
                pp_ = tg % 2
                for cc in range(2):
                    ps, rp = ppw.get()
                    for c in range(16):
                        mm(ps, wu[:, c, cc * 128:(cc + 1) * 128], hT[:, c, t0:t0 + 512], c == 0, c == 15, [r_wu, r_hT], [rp])
                    buf = ub[cc][pp_]
                    rb_ = r_ub[cc][pp_]
                    cp("act", buf[:, 16:528], ps, [rp], [rb_])
                    if tg == 0:
                        memset("dve", buf[:, 0:16], 0.0, [rb_])
                    else:
                        cp("dve", buf[:, 0:16], ub[cc][1 - pp_][:, 512:528], [r_ub[cc][1 - pp_]], [rb_])
                    src, rsrc = buf, rb_
                    k = 1
                    tog = 0
                    while k < wlen:
                        dst, rdst = (wsA, r_wsA) if tog == 0 else (wsB, r_wsB)
                        tt("dve", dst[:, k:528], src[:, k:528], src[:, 0:528 - k], ALU.add, [rsrc], [rdst])
                        if k > 1:
                            pass
                        src, rsrc = dst, rdst
                        k *= 2
                        tog = 1 - tog
                    stt("dve", dfT[cc], src[:, 16:528], float(1.0 / wlen), buf[:, 16:528], ALU.mult, ALU.subtract, [rsrc, rb_], [r_df[cc]])
                    if tg == 0:
                        tt("dve", t16, src[:, 16:32], invc[:, g, :], ALU.mult, [rsrc, r_c], [r_t16])
                        tt("dve", dfT[cc][:, 0:16], t16, buf[:, 16:32], ALU.subtract, [r_t16, rb_], [r_df[cc]])
                for ddc in range(2):
                    ps, rp = ppw.get()
                    for cc in range(2):
                        mm(ps, pw[:, cc, ddc * 128:(ddc + 1) * 128], dfT[cc], cc == 0, cc == 1, [r_pw, r_df[cc]], [rp])
                    j = g * 2 + ddc
                    ts("dve", ypl[ddc], ps, pscale[:, j:j + 1], None, ALU.mult, None, [rp, r_small], [r_ypl[ddc]])
                    dma("sp", yT_d[j, :, t0:t0 + 512], ypl[ddc], [r_ypl[ddc]], [r_yT])
        P.barrier()

        Wout = R1
        r_W = [Res() for _ in range(16)]
        ar = Arena(R3[:, :])
        gate_b = ar.get([128, 2048], F32)
        gtmp = ar.get([128, 2048], F32)
        r_gate = Res()
        r_gtmp = Res()
        dma("sp", gate_b, mod_d[2 * D:3 * D].partition_broadcast(128), [r_mod], [r_gate])
        dma("sp", gtmp, b_ada[2 * D:3 * D].partition_broadcast(128), (), [r_gtmp])
        tt("dve", gate_b, gate_b, gtmp, ALU.add, [r_gate, r_gtmp], [r_gate])
        for c in range(16):
            dma("pool", Wout[:, c, :], w_out[c * 128:(c + 1) * 128, :], (), [r_W[c]])
            tt("pool" if c % 2 else "dve", Wout[:, c, :], Wout[:, c, :], gate_b, ALU.mult, [r_W[c], r_gate], [r_W[c]])
        YH = R2
        r_YH = [Res() for _ in range(NT)]
        for tti in range(NT):
            dma("sp", YH[:, tti, :, :], yT_d[:, :, tti * 128:(tti + 1) * 128].rearrange("c p q -> p c q"), [r_yT], [r_YH[tti]])
        P.barrier()
        ar = Arena(R3[:, :])
        xb = [ar.get([128, 2048], F32) for _ in range(2)]
        r_xb = [Res(), Res()]
        xn = ar.get([128, 2048], F32)
        xn2_ = ar.get([128, 2048], F32)
        r_xn = Res()
        junk = ar.get([128, 2048], BF16)
        r_junk = Res()
        h2T = [ar.get([128, 128], F32) for _ in range(4)]
        r_h2T = [Res() for _ in range(4)]
        wr = ar.get([128, 16, 36], F32)
        r_wr = Res()
        dma("sp", wr[:, :, 0:4], w_rg.rearrange("(p c) n -> p c n", c=16), (), [r_wr])
        dma("sp", wr[:, :, 4:36], w_re.rearrange("(p c) n -> p c n", c=16), (), [r_wr])
        dma("sp", br_b[:, 0:4], b_rg.partition_broadcast(128), (), [r_small])
        dma("sp", br_b[:, 4:36], b_re.partition_broadcast(128), (), [r_small])
        lg = ar.get([128, 36], F32)
        rt = ar.get([128, 64], F32)
        og = ar.get([128, 4], F32)
        lem = ar.get([128, 32], F32)
        oh1 = ar.get([128, 32], F32)
        oh2 = ar.get([128, 32], F32)
        ohs = ar.get([128, 32], F32)
        pos = ar.get([128, 32], F32)
        cnt_b = ar.get([128, 32], F32)
        tmp32 = ar.get([128, 32], F32)
        pos_all = sb("pos_all", [128, 16, 32], F32)
        oh1_all = sb("oh1_all", [128, 16, 32], F32)
        oh2_all = sb("oh2_all", [128, 16, 32], F32)
        destf = sb("destf", [128, 16, 2], F32)
        desti = sb("desti", [128, 16, 2], I32)
        cnt_i = sb("cnt_i", [128, 32], I32)
        destib = sb("destib", [128, 16, 2], I32)
        wts = sb("wts", [128, 16, 2], F32)
        r_rt = Res()
        r_route = Res()
        memset("dve", cnt_b, 0.0, [r_rt])
        ppw = PsPool([0, 1, 2, 3], 512)
        ppn = PsPool([4, 5, 6], 128)
        pplg = PsPool([7], 128)
        xns = [xn, xn2_]
        r_xns = [r_xn, Res()]

        def mixA(tti):
            b = tti % 2
            xn = xns[b]
            r_xn = r_xns[b]
            dma("sp", xb[b], x_d[tti * 128:(tti + 1) * 128, :], (), [r_xb[b]])
            banks = []
            for nb in range(4):
                ps, rp = ppw.get()
                for c in range(16):
                    mm(ps, YH[:, tti, c, :], Wout[:, c, nb * 512:(nb + 1) * 512], c == 0, c == 15, [r_YH[tti], r_W[c]], [rp])
                banks.append((ps, rp))
            for nb in range(4):
                ps, rp = banks[nb]
                tt("dve", xb[b][:, nb * 512:(nb + 1) * 512], ps, xb[b][:, nb * 512:(nb + 1) * 512], ALU.add, [rp, r_xb[b]], [r_xb[b]])
            dma("sp", x1_d[tti * 128:(tti + 1) * 128, :], xb[b], [r_xb[b]], [r_x1[tti]])
            act(junk, xb[b], AF.Square, [r_xb[b]], [r_junk, r_ss], accum=ss_col[:, 0:1])
            rsqrt_act(ss_col[:, 2:3], ss_col[:, 0:1], 1.0 / D, [r_ss], [r_ss], ss_col[:, 1:2], r_ss)
            ts("dve", xn, xb[b], ss_col[:, 2:3], None, ALU.mult, None, [r_xb[b], r_ss], [r_xn])
            cp("pool", YH[:, tti, :, :].rearrange("t c p -> t (c p)"), xn, [r_xn], [r_YH[tti]])

        def postB(tti):
            b = tti % 2
            xn = xns[b]
            r_xn = r_xns[b]
            xnv = xn.rearrange("t (p c) -> t c p", c=16)
            psl, rpl = pplg.get()
            for c in range(16):
                ps, rp = ppn.get()
                tr(ps, xnv[:, c, :], ident_f, [r_xn, r_c], [rp])
                hb = h2T[c % 4]
                rhb = r_h2T[c % 4]
                if c % 2 == 0:
                    act(hb, ps, AF.Identity, [rp, r_small], [rhb], scale=a2c[:, c:c + 1], bias=b2c[:, c:c + 1])
                else:
                    ts("dve", hb, ps, a2c[:, c:c + 1], b2c[:, c:c + 1], ALU.mult, ALU.add, [rp, r_small], [rhb])
                mm(psl[:, 0:36], hb, wr[:, c, :], c == 0, c == 15, [rhb, r_wr], [rpl])
            R_ = [r_rt]
            tt("dve", lg, psl[:, 0:36], br_b, ALU.add, [rpl, r_small, r_rt], R_)
            P.op("dve", lambda e: e.reduce_max(out=rt[:, 0:1], in_=lg[:, 0:4], axis=mybir.AxisListType.X), R_, R_)
            ts("dve", og, lg[:, 0:4], rt[:, 0:1], None, ALU.is_equal, None, R_, R_)
            ts("dve", rt[:, 1:2], rt[:, 0:1], -1.0, None, ALU.mult, None, R_, R_)
            act(rt[:, 4:8], lg[:, 0:4], AF.Exp, R_, R_, bias=rt[:, 1:2], accum=rt[:, 2:3])
            P.op("dve", lambda e: e.reciprocal(out=rt[:, 3:4], in_=rt[:, 2:3]), R_, R_)
            for g in range(4):
                ts("dve", rt[:, 8:9], og[:, g:g + 1], 1.0, 1.0e4, ALU.subtract, ALU.mult, R_, R_)
                ts("dve", lem[:, g * 8:(g + 1) * 8], lg[:, 4 + g * 8:12 + g * 8], rt[:, 8:9], None, ALU.add, None, R_, R_)
            P.op("dve", lambda e: e.reduce_max(out=rt[:, 9:10], in_=lem, axis=mybir.AxisListType.X), R_, R_)
            ts("dve", oh1, lem, rt[:, 9:10], None, ALU.is_equal, None, R_, R_)
            stt("dve", tmp32, oh1, -1.0e4, lem, ALU.mult, ALU.add, R_, R_)
            P.op("dve", lambda e: e.reduce_max(out=rt[:, 10:11], in_=tmp32, axis=mybir.AxisListType.X), R_, R_)
            ts("dve", oh2, tmp32, rt[:, 10:11], None, ALU.is_equal, None, R_, R_)
            tt("dve", ohs, oh1, oh2, ALU.add, R_, R_)
            tt("dve", rt[:, 11:12], rt[:, 10:11], rt[:, 9:10], ALU.subtract, R_, R_)
            act(rt[:, 12:13], rt[:, 11:12], AF.Exp, R_, R_)
            ts("dve", rt[:, 12:13], rt[:, 12:13], 1.0, None, ALU.add, None, R_, R_)
            P.op("dve", lambda e: e.reciprocal(out=rt[:, 13:14], in_=rt[:, 12:13]), R_, R_)
            tt("dve", wts[:, tti, 0:1], rt[:, 13:14], rt[:, 3:4], ALU.mult, R_, R_ + [r_route])
            tt("dve", wts[:, tti, 1:2], rt[:, 3:4], wts[:, tti, 0:1], ALU.subtract, R_ + [r_route], R_ + [r_route])
            psp, rpp = ppn.get()
            mm(psp[:, 0:32], CF("ustr"), ohs, True, True, [r_c, r_rt], [rpp])
            tt("dve", pos_all[:, tti, :], psp[:, 0:32], cnt_b, ALU.add, [rpp] + R_, R_ + [r_route])
            psc, rpc = ppn.get()
            mm(psc[:, 0:32], ones_f, ohs, True, True, [r_c, r_rt], [rpc])
            tt("dve", cnt_b, cnt_b, psc[:, 0:32], ALU.add, [rpc] + R_, R_)
            cp("dve", oh1_all[:, tti, :], oh1, R_, R_ + [r_route])
            cp("dve", oh2_all[:, tti, :], oh2, R_, R_ + [r_route])
        mixA(0)
        for tti in range(NT):
            if tti + 1 < NT:
                mixA(tti + 1)
            postB(tti)
        RR = [r_rt, r_route]
        for tti in range(NT):
            tt("dve", tmp32, pos_all[:, tti, :], CF("eoff"), ALU.add, RR + [r_c], RR)
            tt("dve", pos, tmp32, oh1_all[:, tti, :], ALU.mult, RR, RR)
            P.op("dve", lambda e, tti=tti: e.reduce_sum(out=destf[:, tti, 0:1], in_=pos, axis=mybir.AxisListType.X), RR, RR)
            tt("dve", pos, tmp32, oh2_all[:, tti, :], ALU.mult, RR, RR)
            P.op("dve", lambda e, tti=tti: e.reduce_sum(out=destf[:, tti, 1:2], in_=pos, axis=mybir.AxisListType.X), RR, RR)
        cp("dve", desti[:, :, :].rearrange("p a b -> p (a b)"), destf[:, :, :].rearrange("p a b -> p (a b)"), RR, RR)
        cp("dve", cnt_i[:, :], cnt_b, RR, RR)
        dfl = destf[:, :, :].rearrange("p a b -> p (a b)")
        dfb = ar.get([128, 32], F32)
        ts("dve", dfb, dfl, 32768.0, 100000.0, ALU.is_lt, ALU.mult, RR, RR)
        stt("dve", dfb, dfl, -32768.0, dfb, ALU.add, ALU.add, RR, RR)
        cp("dve", destib[:, :, :].rearrange("p a b -> p (a b)"), dfb, RR, RR)
        P.barrier()

        if stage == 2:
            for tti in range(NT):
                b = tti % 2
                dma("sp", xb[b], x1_d[tti * 128:(tti + 1) * 128, :], [r_x1[tti]], [r_xb[b]])
                dma("sp", out_d[tti * 128:(tti + 1) * 128, :], xb[b], [r_xb[b]], ())
            P.emit(nc, es.enter_context(nc.Block()), sems)
            return nc

        NK = 16
        r2flat = R2[:, :, :, :].rearrange("p a b c -> p (a b c)")
        r3flat = R3[:, :]
        zt = r3flat[:, 0:2048]
        r_zt = Res()
        memset("dve", zt, 0.0, [r_zt])
        for e_ in range(NE):
            for k in range(NK):
                P.begin_cond(cnt_i[0:1, e_:e_ + 1], k * 128 + 1)
                row0 = e_ * 2048 + k * 128
                dma("sp", xs_d[row0:row0 + 128, :], zt, [r_zt], [Res()])
            for k in range(NK):
                P.end_cond()
        P.barrier()
        for tti in range(NT):
            for k in range(2):
                P.dma("pool", lambda e, tti=tti, k=k: e.indirect_dma_start(
                    out=xs_d, out_offset=bass.IndirectOffsetOnAxis(ap=desti[:, tti, k:k + 1], axis=0),
                    in_=YH[:, tti, :, :].rearrange("t c p -> t (c p)"), in_offset=None), [r_YH[tti], r_route], [Res()])
        P.barrier()

        def v3(base, o, n):
            return base[:, o:o + 12288].rearrange("p (c n) -> p c n", n=n)

        def vf(base, o, nel, shape=None, dt=BF16):
            a_ = base[:, o:o + nel]
            if dt != BF16:
                a_ = a_.bitcast(dt)
            if shape is not None:
                a_ = a_.rearrange("p (a b) -> p a b", b=shape)
            return a_

        wsets = [[v3(r1flat, 0, DE), v3(r1flat, 12288, DE), v3(r2flat, 0, D)],
                 [v3(r2flat, 12288, DE), v3(r3flat, 0, DE), v3(r3flat, 12288, D)]]
        r_wset = [[Res() for _ in range(3)] for _ in range(2)]
        yo = [vf(r1flat, 24576, 4096, dt=F32), vf(r1flat, 28672, 4096, dt=F32)]
        r_yo = [Res(), Res()]
        xblk = [vf(r2flat, 24576, 2048), vf(r2flat, 26624, 2048), vf(r3flat, 26624, 2048)]
        r_xblk = [Res(), Res(), Res()]
        XT = [vf(r2flat, 28672, 2048, 128), vf(r2flat, 30720, 2048, 128)]
        r_XT = [[Res() for _ in range(16)] for _ in range(2)]
        Hs = [vf(r3flat, 24576, 256, dt=F32), vf(r3flat, 24832, 256, dt=F32)]
        r_Hs = [Res(), Res()]
        Hh = [vf(r3flat, 25088, 768, 128), vf(r3flat, 25856, 768, 128)]
        r_Hh = [Res(), Res()]
        ppd = PsPool([5, 6, 7], 512)
        srcs = [(w_gate, "(p c) n -> p c n", {"c": 16}), (w_up, "(p c) n -> p c n", {"c": 16}), (w_down, "(c p) n -> p c n", {"p": 128})]
        bi = 0
        gi = 0
        for e_ in range(NE):
            ws = e_ % 2
            for which in range(3):
                wsrc, pat, kw = srcs[which]
                dma("pool", wsets[ws][which], wsrc[e_].rearrange(pat, **kw), (), [r_wset[ws][which]])
            Wg, Wu_, Wd = wsets[ws]
            rWg, rWu, rWd = r_wset[ws]
            for k in range(NK):
                P.begin_cond(cnt_i[0:1, e_:e_ + 1], k * 128 + 1)
                st = bi % 2
                bi += 1
                row0 = e_ * 2048 + k * 128
                sx = (bi - 1) % 3
                xb_ = xblk[sx]
                dma("act", xb_, xs_d[row0:row0 + 128, :], [r_xs], [r_xblk[sx]])
                xv = xb_.rearrange("t (p c) -> t c p", c=16)
                for h in range(2):
                    rp = bank_res[h]
                    for j in range(8):
                        c = h * 8 + j
                        tr(psum[:, h, j * 64:(j + 1) * 64].bitcast(BF16), xv[:, c, :], ident_b, [r_xblk[sx], r_c], [rp])
                    for j in range(8):
                        c = h * 8 + j
                        src_ = psum[:, h, j * 64:(j + 1) * 64].bitcast(BF16)
                        if j % 2 == 0:
                            act(XT[st][:, c, :], src_, AF.Identity, [rp, r_small], [r_XT[st][c]], scale=a2c[:, c:c + 1], bias=b2c[:, c:c + 1])
                        else:
                            ts("dve", XT[st][:, c, :], src_, a2c[:, c:c + 1], b2c[:, c:c + 1], ALU.mult, ALU.add, [rp, r_small], [r_XT[st][c]])
                for hc in range(6):
                    gb = 2 + gi % 3
                    gi += 1
                    rpg = bank_res[gb]
                    psg = psum[:, gb, 0:128]
                    psu = psum[:, gb, 128:256]
                    for c in range(16):
                        mm(psg, Wg[:, c, hc * 128:(hc + 1) * 128], XT[st][:, c, :], c == 0, c == 15, [rWg, r_XT[st][c]], [rpg])
                    for c in range(16):
                        mm(psu, Wu_[:, c, hc * 128:(hc + 1) * 128], XT[st][:, c, :], c == 0, c == 15, [rWu, r_XT[st][c]], [rpg])
                    hb = hc % 2
                    act(Hs[hb], psg, AF.Silu, [rpg], [r_Hs[hb]])
                    tt("dve", Hh[st][:, hc, :], Hs[hb], psu, ALU.mult, [r_Hs[hb], rpg], [r_Hh[st]])
                for nb in range(4):
                    ps, rp = ppd.get()
                    for hc in range(6):
                        mm(ps, Hh[st][:, hc, :], Wd[:, hc, nb * 512:(nb + 1) * 512], hc == 0, hc == 5, [r_Hh[st], rWd], [rp])
                    if nb % 2 == 0:
                        cp("act", yo[st][:, nb * 512:(nb + 1) * 512], ps, [rp], [r_yo[st]])
                    else:
                        cp("dve", yo[st][:, nb * 512:(nb + 1) * 512], ps, [rp], [r_yo[st]])
                dma("sp", ys_a[row0:row0 + 128, :], yo[st][:, 0:1024], [r_yo[st]], [r_ys])
                dma("sp", ys_b[row0:row0 + 128, :], yo[st][:, 1024:2048], [r_yo[st]], [r_ys])
            for k in range(NK):
                P.end_cond()
        P.barrier()

        ar = Arena(R3[:, :])
        g2b = ar.get([128, 2048], F32)
        nfb = ar.get([128, 2048], F32)
        r_g2b = Res()
        r_nfb = Res()
        yas = [ar.get([128, 2048], F32) for _ in range(2)]
        ybufs = [ar.get([128, 2048], F32) for _ in range(2)]
        r_yas = [Res(), Res()]
        r_yb2s = [Res(), Res()]
        ar1 = Arena(r1flat)
        x1t = [ar1.get([128, 2048], F32) for _ in range(2)]
        r_x1t = [Res(), Res()]
        gt2 = ar1.get([128, 2048], F32)
        r_gt2 = Res()
        junk = ar1.get([128, 2048], BF16)
        dma("sp", g2b, mod_d[5 * D:6 * D].partition_broadcast(128), [r_mod], [r_g2b])
        dma("sp", gt2, b_ada[5 * D:6 * D].partition_broadcast(128), (), [r_gt2])
        tt("dve", g2b, g2b, gt2, ALU.add, [r_g2b, r_gt2], [r_g2b])
        dma("sp", nfb, norm_f_g.partition_broadcast(128), (), [r_nfb])
        for tti in range(NT):
            b = tti % 2
            ya, ybuf, r_ya, r_yb2 = yas[b], ybufs[b], r_yas[b], r_yb2s[b]
            for (buf_, rbuf_, k_) in ((ya, r_ya, 0), (ybuf, r_yb2, 1)):
                P.dma("pool", lambda e, tti=tti, buf_=buf_, k_=k_: e.indirect_dma_start(
                    out=buf_[:, 0:1024], out_offset=None, in_=ys_a,
                    in_offset=bass.IndirectOffsetOnAxis(ap=desti[:, tti, k_:k_ + 1], axis=0)), [r_ys, r_route], [rbuf_])
                P.dma("pool", lambda e, tti=tti, buf_=buf_, k_=k_: e.indirect_dma_start(
                    out=buf_[:, 1024:2048], out_offset=None, in_=ys_b,
                    in_offset=bass.IndirectOffsetOnAxis(ap=desti[:, tti, k_:k_ + 1], axis=0)), [r_ys, r_route], [rbuf_])
            dma("sp", x1t[b], x1_d[tti * 128:(tti + 1) * 128, :], [r_x1[tti]], [r_x1t[b]])
            ts("dve", ya, ya, wts[:, tti, 0:1], None, ALU.mult, None, [r_ya, r_route], [r_ya])
            stt("dve", ya, ybuf, wts[:, tti, 1:2], ya, ALU.mult, ALU.add, [r_yb2, r_route, r_ya], [r_ya])
            tt("pool", ya, ya, g2b, ALU.mult, [r_ya, r_g2b], [r_ya])
            tt("dve", x1t[b], x1t[b], ya, ALU.add, [r_x1t[b], r_ya], [r_x1t[b]])
            act(junk, x1t[b], AF.Square, [r_x1t[b]], [r_junk, r_ss], accum=ss_col[:, 0:1])
            rsqrt_act(ss_col[:, 2:3], ss_col[:, 0:1], 1.0 / D, [r_ss], [r_ss], ss_col[:, 1:2], r_ss)
            stt("dve", x1t[b], x1t[b], ss_col[:, 2:3], nfb, ALU.mult, ALU.mult, [r_x1t[b], r_ss, r_nfb], [r_x1t[b]])
            dma("sp", out_d[tti * 128:(tti + 1) * 128, :], x1t[b], [r_x1t[b]], ())

        P.emit(nc, es.enter_context(nc.Block()), sems)
    return nc


_INKEYS = ["w_ada", "b_ada", "norm1_g", "w_in", "pool_w", "pool_scale", "conv_w", "a_log", "dt_bias",
           "o_norm_g", "w_out", "norm2_g", "w_gate", "w_up", "w_down"]


def make_in_maps(inputs, cores):
    shared = {k: np.ascontiguousarray(np.asarray(inputs[k])[0]) for k in _INKEYS}
    shared["w_rg"] = np.ascontiguousarray(np.asarray(inputs["w_router_group"])[0])
    shared["b_rg"] = np.ascontiguousarray(np.asarray(inputs["b_router_group"])[0])
    shared["w_re"] = np.ascontiguousarray(np.asarray(inputs["w_router_expert"])[0])
    shared["b_re"] = np.ascontiguousarray(np.asarray(inputs["b_router_expert"])[0])
    shared["norm_f_g"] = np.ascontiguousarray(np.asarray(inputs["norm_f_g"]))
    shared["consts"] = CONSTS
    x = np.asarray(inputs["x"])
    c = np.asarray(inputs["c"])
    maps = []
    for b in cores:
        m = dict(shared)
        m["x"] = np.ascontiguousarray(x[b])
        m["c"] = np.ascontiguousarray(c[b].reshape(128, 16))
        maps.append(m)
    return maps


def kernel(**inputs):
    nc = build()
    maps = make_in_maps(inputs, list(range(8)))
    res = run_bass_kernel_spmd(nc, maps, core_ids=list(range(8)))
    return np.stack([np.asarray(r["out"], dtype=np.float32) for r in res.results], axis=0)
```
